# Optimizing a Trainium2 kernel written in Bass

```python
import jax, jax.numpy as jnp
from jax import lax
import numpy as np


D_MODEL = 1024
BATCH = 4
SEQ = 4096
DEPTH = 2

ATT_GROUPS = ((128, 1), (512, 4), (2048, 16))
ATT_HEADS_PER_GROUP = 4
ATT_HEAD_DIM = 64
ATT_HEADS = len(ATT_GROUPS) * ATT_HEADS_PER_GROUP
ATT_WIDTH = ATT_HEADS * ATT_HEAD_DIM
ATT_OUT_WIDTH = ATT_HEADS_PER_GROUP * ATT_HEAD_DIM
ROPE_THETA = 10000.0
SG_CHUNK = 128
SG_GROUPS = 6
SG_WIDTH = 768
SG_GROUP_DIM = SG_WIDTH // SG_GROUPS
ML_HEADS = 4
ML_HEAD_DIM = 192
ML_WIDTH = ML_HEADS * ML_HEAD_DIM
ML_CHUNK = 128
ML_CONV = 4
N_BRANCH = 3
IN_SIZES = (ATT_WIDTH, ATT_WIDTH, ATT_WIDTH, SG_WIDTH, SG_WIDTH, 2 * ML_WIDTH, ML_WIDTH, ML_WIDTH, ML_HEADS, ML_HEADS, N_BRANCH * D_MODEL)
N_IN = 9992
F_GATE_OFFSET = 6148
D_FF = 2816
N_EXPERTS = 8
TOP_K = 2
D_FF_EXPERT = 2816
N_DENSE = (DEPTH + 1) // 2
N_MOE = DEPTH // 2
LN_EPS = 1e-5

kernel_name = 'hybrid_dilated_gmlp_mlstm_moe_deepnorm'


def layer_norm(x, g, b):
    xf = x.astype(jnp.float32)
    mu = xf.mean(-1, keepdims=True)
    var = jnp.square(xf - mu).mean(-1, keepdims=True)
    return ((xf - mu) * lax.rsqrt(var + LN_EPS) * g + b).astype(x.dtype)


def rotary(x, positions):
    dh = x.shape[-1]
    half = dh // 2
    freqs = ROPE_THETA ** (-jnp.arange(half, dtype=jnp.float32) * (2.0 / dh))
    ang = positions.astype(jnp.float32)[..., None] * freqs
    cos = jnp.cos(ang)[:, :, None, :]
    sin = jnp.sin(ang)[:, :, None, :]
    xf = x.astype(jnp.float32)
    x1, x2 = xf[..., :half], xf[..., half:]
    return jnp.concatenate([x1 * cos - x2 * sin, x2 * cos + x1 * sin], axis=-1).astype(x.dtype)


def dilated_window_group(q, k, v, window, dilation):
    B, S, H, Dh = q.shape
    steps = window // dilation
    L = S // dilation
    nb = -(-L // steps)
    Lp = nb * steps

    def to_blocks(t):
        t = t.reshape(B, L, dilation, H, Dh).transpose(0, 2, 3, 1, 4)
        t = jnp.pad(t, ((0, 0), (0, 0), (0, 0), (0, Lp - L), (0, 0)))
        return t.reshape(B, dilation, H, nb, steps, Dh)

    def frame(t):
        prev = jnp.pad(t, ((0, 0), (0, 0), (0, 0), (1, 0), (0, 0), (0, 0)))[:, :, :, :nb]
        return jnp.concatenate([prev, t], axis=4)

    qb = to_blocks(q)
    kw = frame(to_blocks(k))
    vw = frame(to_blocks(v))
    s = jnp.einsum('brhnqd,brhnkd->brhnqk', qb, kw).astype(jnp.float32) * (Dh ** -0.5)
    qi = jnp.arange(steps)[:, None] + steps
    ki = jnp.arange(2 * steps)[None, :]
    dist = qi - ki
    k_abs = jnp.arange(nb)[:, None, None] * steps - steps + ki[None]
    valid = (dist >= 0) & (dist <= steps) & (k_abs >= 0)
    s = jnp.where(valid, s, -jnp.inf)
    m = s.max(-1, keepdims=True)
    p = jnp.exp(s - m)
    den = p.sum(-1, keepdims=True)
    o = jnp.einsum('brhnqk,brhnkd->brhnqd', (p / den).astype(v.dtype), vw)
    lse = (m + jnp.log(den))[..., 0]
    o = o.reshape(B, dilation, H, Lp, Dh)[:, :, :, :L].transpose(0, 3, 1, 2, 4).reshape(B, S, H, Dh)
    lse = lse.reshape(B, dilation, H, Lp)[:, :, :, :L].transpose(0, 3, 1, 2).reshape(B, S, H)
    return o, lse


def spatial_gating(u, v, ln_g, ln_b, w_s, b_s):
    B, S, _ = v.shape
    v = layer_norm(v, ln_g, ln_b)
    vc = v.reshape(B, S // SG_CHUNK, SG_CHUNK, SG_GROUPS, SG_GROUP_DIM)
    w = jnp.tril(w_s)
    mixed = jnp.einsum('gts,bcsgd->bctgd', w, vc) + b_s.T[:, :, None]
    return u * mixed.reshape(B, S, SG_WIDTH)


def causal_depthwise_conv(x, w, b):
    K = w.shape[0]
    S = x.shape[1]
    xp = jnp.pad(x, ((0, 0), (K - 1, 0), (0, 0)))
    out = xp[:, 0:S] * w[0]
    for j in range(1, K):
        out = out + xp[:, j:j + S] * w[j]
    return out + b


def mlstm_chunkwise(q, k, v, i_pre, f_pre):
    B, S, H, D = q.shape
    nc = S // ML_CHUNK
    L = ML_CHUNK

    def chunks(t):
        return t.astype(jnp.float32).reshape(B, nc, L, H, D).transpose(1, 0, 3, 2, 4)

    def gchunks(t):
        return t.astype(jnp.float32).reshape(B, nc, L, H).transpose(1, 0, 3, 2)

    qc = chunks(q)
    kc = chunks(k) * (D ** -0.5)
    vc = chunks(v)
    ic = gchunks(i_pre)
    lfc = jax.nn.log_sigmoid(gchunks(f_pre))
    causal = jnp.tril(jnp.ones((L, L), dtype=bool))

    def step(carry, xs):
        C, n, m_prev = carry
        qx, kx, vx, ix, lfx = xs
        b = jnp.cumsum(lfx, axis=-1)
        log_d = jnp.where(causal, b[..., :, None] - b[..., None, :] + ix[..., None, :], -jnp.inf)
        m_inter = b + m_prev[..., None]
        m = jnp.maximum(m_inter, log_d.max(-1))
        sc = jnp.einsum('bhtd,bhsd->bhts', qx, kx) * jnp.exp(log_d - m[..., None])
        inter = jnp.exp(m_inter - m)
        num = jnp.einsum('bhts,bhsd->bhtd', sc, vx) + inter[..., None] * jnp.einsum('bhvk,bhtk->bhtv', C, qx)
        den = sc.sum(-1) + inter * jnp.einsum('bhk,bhtk->bht', n, qx)
        h = num / jnp.maximum(jnp.abs(den), jnp.exp(-m))[..., None]
        m_new = m[..., -1]
        w = jnp.exp(b[..., -1:] - b + ix - m_new[..., None])
        decay = jnp.exp(b[..., -1] + m_prev - m_new)
        C = decay[..., None, None] * C + jnp.einsum('bhs,bhsv,bhsk->bhvk', w, vx, kx)
        n = decay[..., None] * n + jnp.einsum('bhs,bhsk->bhk', w, kx)
        return (C, n, m_new), h

    init = (jnp.zeros((B, H, D, D), jnp.float32), jnp.zeros((B, H, D), jnp.float32), jnp.zeros((B, H), jnp.float32))
    _, h = lax.scan(step, init, (qc, kc, vc, ic, lfc))
    return h.transpose(1, 0, 3, 2, 4).reshape(B, S, H * D)


def token_mixer(x, positions, w_in, b_in, conv_w, conv_b, sg_ln_g, sg_ln_b, sg_w, sg_b, w_br_a, w_br_b, w_br_c, w_out):
    B, S, _ = x.shape
    splits = [int(c) for c in np.cumsum(IN_SIZES)[:-1]]
    h = x @ w_in + b_in
    a_q, a_k, a_v, b_u, b_v, c_qk, c_v, c_o, c_i, c_f, g_pre = jnp.split(h, splits, axis=-1)

    q = rotary(a_q.reshape(B, S, ATT_HEADS, ATT_HEAD_DIM), positions)
    k = rotary(a_k.reshape(B, S, ATT_HEADS, ATT_HEAD_DIM), positions)
    v = a_v.reshape(B, S, ATT_HEADS, ATT_HEAD_DIM)
    outs, lses = [], []
    for gi, (win, dil) in enumerate(ATT_GROUPS):
        sl = slice(gi * ATT_HEADS_PER_GROUP, (gi + 1) * ATT_HEADS_PER_GROUP)
        o, l = dilated_window_group(q[:, :, sl], k[:, :, sl], v[:, :, sl], win, dil)
        outs.append(o)
        lses.append(l)
    wts = jax.nn.softmax(jnp.stack(lses), axis=0)
    y_a = jnp.einsum('gbsh,gbshd->bshd', wts.astype(x.dtype), jnp.stack(outs)).reshape(B, S, ATT_OUT_WIDTH)

    y_b = spatial_gating(jax.nn.gelu(b_u), jax.nn.gelu(b_v), sg_ln_g, sg_ln_b, sg_w, sg_b)

    qk = jax.nn.silu(causal_depthwise_conv(c_qk, conv_w, conv_b))
    c_q, c_k = jnp.split(qk, 2, axis=-1)
    heads = lambda t: t.reshape(B, S, ML_HEADS, ML_HEAD_DIM)
    h_c = mlstm_chunkwise(heads(c_q), heads(c_k), heads(c_v), c_i, c_f)
    y_c = jax.nn.sigmoid(c_o) * h_c.astype(x.dtype)

    gates = jax.nn.sigmoid(g_pre.reshape(B, S, N_BRANCH, D_MODEL))
    z = gates[:, :, 0] * (y_a @ w_br_a) + gates[:, :, 1] * (y_b @ w_br_b) + gates[:, :, 2] * (y_c @ w_br_c)
    return z @ w_out


def swiglu(x, w1, w3, w2):
    return (jax.nn.silu(x @ w1) * (x @ w3)) @ w2


def moe_swiglu(x, router_w, router_b, w1, w3, w2):
    B, S, D = x.shape
    xt = x.reshape(-1, D)
    logits = (xt @ router_w).astype(jnp.float32) + router_b
    top_v, top_i = lax.top_k(logits, TOP_K)
    top_w = jax.nn.softmax(top_v, axis=-1)
    gates = jnp.einsum('nk,nke->ne', top_w, jax.nn.one_hot(top_i, N_EXPERTS, dtype=jnp.float32)).astype(xt.dtype)
    out = jnp.zeros_like(xt)
    for e in range(N_EXPERTS):
        out = out + gates[:, e:e + 1] * swiglu(xt, w1[e], w3[e], w2[e])
    return out.reshape(B, S, D)


def setup_inputs(seed: int = 0) -> dict:
    key = jax.random.key(seed)
    ks = jax.random.split(key, 26)
    nrm = lambda k, shape, scale: jax.random.normal(k, shape, jnp.float32) * scale
    beta = (8.0 * DEPTH) ** -0.25
    x = nrm(ks[0], (BATCH, SEQ, D_MODEL), 1.0)
    start = jax.random.randint(ks[1], (BATCH, 1), 0, 1024, dtype=jnp.int32)
    positions = start + jnp.arange(SEQ, dtype=jnp.int32)[None, :]
    w_in = nrm(ks[2], (DEPTH, D_MODEL, N_IN), D_MODEL ** -0.5)
    b_in = nrm(ks[3], (DEPTH, N_IN), 0.02)
    b_in = b_in.at[:, F_GATE_OFFSET:F_GATE_OFFSET + ML_HEADS].add(jnp.linspace(3.0, 6.0, ML_HEADS, dtype=jnp.float32))
    conv_w = nrm(ks[4], (DEPTH, ML_CONV, 2 * ML_WIDTH), ML_CONV ** -0.5)
    conv_b = nrm(ks[5], (DEPTH, 2 * ML_WIDTH), 0.02)
    sg_ln_g = 1.0 + nrm(ks[6], (DEPTH, SG_WIDTH), 0.02)
    sg_ln_b = nrm(ks[7], (DEPTH, SG_WIDTH), 0.02)
    sg_w = nrm(ks[8], (DEPTH, SG_GROUPS, SG_CHUNK, SG_CHUNK), SG_CHUNK ** -0.5)
    sg_b = 1.0 + nrm(ks[9], (DEPTH, SG_GROUPS, SG_CHUNK), 0.02)
    w_br_a = nrm(ks[10], (DEPTH, ATT_OUT_WIDTH, D_MODEL), ATT_OUT_WIDTH ** -0.5)
    w_br_b = nrm(ks[11], (DEPTH, SG_WIDTH, D_MODEL), SG_WIDTH ** -0.5)
    w_br_c = nrm(ks[12], (DEPTH, ML_WIDTH, D_MODEL), ML_WIDTH ** -0.5)
    w_out = nrm(ks[13], (DEPTH, D_MODEL, D_MODEL), D_MODEL ** -0.5 * beta)
    ln_g = 1.0 + nrm(ks[14], (DEPTH, 2, D_MODEL), 0.02)
    ln_b = nrm(ks[15], (DEPTH, 2, D_MODEL), 0.02)
    ffn_w1 = nrm(ks[16], (N_DENSE, D_MODEL, D_FF), D_MODEL ** -0.5)
    ffn_w3 = nrm(ks[17], (N_DENSE, D_MODEL, D_FF), D_MODEL ** -0.5)
    ffn_w2 = nrm(ks[18], (N_DENSE, D_FF, D_MODEL), D_FF ** -0.5 * beta)
    router_w = nrm(ks[19], (N_MOE, D_MODEL, N_EXPERTS), D_MODEL ** -0.5)
    router_b = nrm(ks[20], (N_MOE, N_EXPERTS), 0.01)
    moe_w1 = nrm(ks[21], (N_MOE, N_EXPERTS, D_MODEL, D_FF_EXPERT), D_MODEL ** -0.5)
    moe_w3 = nrm(ks[22], (N_MOE, N_EXPERTS, D_MODEL, D_FF_EXPERT), D_MODEL ** -0.5)
    moe_w2 = nrm(ks[23], (N_MOE, N_EXPERTS, D_FF_EXPERT, D_MODEL), D_FF_EXPERT ** -0.5 * beta)
    return {'x': x, 'positions': positions, 'w_in': w_in, 'b_in': b_in, 'conv_w': conv_w, 'conv_b': conv_b,
            'sg_ln_g': sg_ln_g, 'sg_ln_b': sg_ln_b, 'sg_w': sg_w, 'sg_b': sg_b,
            'w_br_a': w_br_a, 'w_br_b': w_br_b, 'w_br_c': w_br_c, 'w_out': w_out,
            'ln_g': ln_g, 'ln_b': ln_b, 'ffn_w1': ffn_w1, 'ffn_w3': ffn_w3, 'ffn_w2': ffn_w2,
            'router_w': router_w, 'router_b': router_b, 'moe_w1': moe_w1, 'moe_w3': moe_w3, 'moe_w2': moe_w2}


def reference(x, positions, w_in, b_in, conv_w, conv_b, sg_ln_g, sg_ln_b, sg_w, sg_b,
              w_br_a, w_br_b, w_br_c, w_out, ln_g, ln_b, ffn_w1, ffn_w3, ffn_w2,
              router_w, router_b, moe_w1, moe_w3, moe_w2):
    alpha = (2.0 * DEPTH) ** 0.25
    for layer in range(DEPTH):
        mix = token_mixer(x, positions, w_in[layer], b_in[layer], conv_w[layer], conv_b[layer],
                          sg_ln_g[layer], sg_ln_b[layer], sg_w[layer], sg_b[layer],
                          w_br_a[layer], w_br_b[layer], w_br_c[layer], w_out[layer])
        x = layer_norm(alpha * x + mix, ln_g[layer, 0], ln_b[layer, 0])
        j = layer // 2
        if layer % 2 == 0:
            f = swiglu(x, ffn_w1[j], ffn_w3[j], ffn_w2[j])
        else:
            f = moe_swiglu(x, router_w[j], router_b[j], moe_w1[j], moe_w3[j], moe_w2[j])
        x = layer_norm(alpha * x + f, ln_g[layer, 1], ln_b[layer, 1])
    return x
```

```python
import math, os
import numpy as np
import ml_dtypes
from contextlib import ExitStack
import concourse.bass as bass
import concourse.mybir as mybir
from concourse.bass_utils import run_bass_kernel_spmd


F32 = mybir.dt.float32
BF16 = mybir.dt.bfloat16
I32 = mybir.dt.int32
AF = mybir.ActivationFunctionType
ALU = mybir.AluOpType
AX = mybir.AxisListType

ENGS = ["sync", "scalar", "vector", "gpsimd", "tensor"]
SAME_ENG_SYNC = {"scalar": True, "vector": True, "gpsimd": True, "tensor": False, "sync": False}


class Prog:
    def __init__(self, nc, stack):
        self.nc = nc
        self.stack = stack
        self.ops = {e: [] for e in ENGS}
        self.res = {}
        self.dma_count = {}
        self.seen = {e: {} for e in ENGS}
        self.nsb = 0
        self.stacks = [stack]
        self.last_real = {}

    def sb(self, shape, dtype, name=None):
        self.nsb += 1
        name = "s_" + getattr(self, "prefix", "") + (name or f"sb{self.nsb}")
        return self.stacks[-1].enter_context(self.nc.sbuf_tensor(name, list(shape), dtype))

    def ps(self, shape, dtype, name=None):
        self.nsb += 1
        name = "p_" + getattr(self, "prefix", "") + (name or f"ps{self.nsb}")
        return self.stacks[-1].enter_context(self.nc.psum_tensor(name, list(shape), dtype))

    def _need(self, eng, tok, waits):
        if tok is None:
            return
        if tok[0] == "e":
            _, e2, idx = tok
            if e2 == eng and not SAME_ENG_SYNC[eng]:
                return
            k = ("e", e2)
            if self.seen[eng].get(k, -1) >= idx:
                return
            if waits.get(k, -1) < idx:
                waits[k] = idx
        else:
            _, key, cnt = tok
            cnt = self.dma_count[key]
            k = ("d", key)
            if self.seen[eng].get(k, -1) >= cnt:
                return
            if waits.get(k, -1) < cnt:
                waits[k] = cnt

    def op(self, eng, fn, reads=(), writes=(), dma_key=None, sem_inc=16):
        excl = [r for r in reads if r.startswith("ps") and r not in writes]
        if excl:
            writes = list(writes) + excl
        waits = {}
        for r in reads:
            st = self.res.get(r)
            if st is not None:
                self._need(eng, st["w"], waits)
        for w in writes:
            st = self.res.get(w)
            if st is not None:
                self._need(eng, st["w"], waits)
                for t in st["r"].values():
                    self._need(eng, t, waits)
        for k, v in waits.items():
            self.seen[eng][k] = v
        idx = len(self.ops[eng])
        if dma_key is not None:
            self.dma_count[dma_key] = self.dma_count.get(dma_key, 0) + 1
            self.key_inc = getattr(self, "key_inc", {})
            self.key_inc[dma_key] = sem_inc
            tok = ("d", dma_key, self.dma_count[dma_key])
            rk = ("d", dma_key)
        else:
            tok = ("e", eng, idx)
            rk = ("e", eng)
        self.ops[eng].append({"fn": fn, "waits": waits, "dma_key": dma_key, "sig": False})
        if fn is not None and dma_key is None:
            self.last_real[eng] = idx
        if fn is not None:
            for r in reads:
                st = self.res.setdefault(r, {"w": None, "r": {}})
                st["r"][rk] = tok
        for w in writes:
            self.res[w] = {"w": tok, "r": {}}
        return tok

    def barrier(self, dma=True):
        toks = [("e", e2, i) for e2, i in self.last_real.items()]
        if dma:
            toks += [("d", k, c) for k, c in self.dma_count.items()]
        for e in ENGS:
            waits = {}
            for t in toks:
                self._need(e, t, waits)
            if waits:
                for k, v in waits.items():
                    self.seen[e][k] = v
                self.ops[e].append({"fn": None, "waits": waits, "dma_key": None, "sig": False})

    def scope(self):
        P = self

        class _S:
            def __enter__(s_):
                s_.st = ExitStack()
                s_.st.__enter__()
                P.stacks.append(s_.st)

            def __exit__(s_, *a):
                P.barrier()
                P.stacks.pop()
                return s_.st.__exit__(*a)
        return _S()

    def push(self):
        st = ExitStack()
        st.__enter__()
        self.stacks.append(st)

    def pop(self, dma=True):
        self.barrier(dma)
        st = self.stacks.pop()
        st.__exit__(None, None, None)

    def wait_res(self, eng, reads):
        self.op(eng, None, reads=reads)

    def emit(self):
        nc = self.nc
        for e in ENGS:
            for o in self.ops[e]:
                for (kind, k), v in o["waits"].items():
                    if kind == "e":
                        self.ops[k][v]["sig"] = True
        sigcnt = {}
        for e in ENGS:
            c = 0
            arr = []
            for o in self.ops[e]:
                if o["sig"]:
                    c += 1
                arr.append(c)
            sigcnt[e] = arr
        esem = {e: self.stack.enter_context(nc.semaphore("es_" + e)) for e in ENGS}
        dsem = {k: self.stack.enter_context(nc.semaphore("ds_%d" % i)) for i, k in enumerate(self.dma_count)}
        self.n_sems = len(esem) + len(dsem)
        block = self.stack.enter_context(nc.Block())

        def make(e):
            def body(eng):
                for o in self.ops[e]:
                    for (kind, k), v in o["waits"].items():
                        if kind == "e":
                            eng.wait_ge(esem[k], sigcnt[k][v])
                        else:
                            eng.wait_ge(dsem[k], self.key_inc[k] * v)
                    if o["fn"] is None:
                        continue
                    inst = o["fn"](eng)
                    if o["dma_key"] is not None:
                        inst.then_inc(dsem[o["dma_key"]], self.key_inc[o["dma_key"]])
                    elif o["sig"]:
                        inst.then_inc(esem[e], 1)
            return body

        for e in ENGS:
            if self.ops[e]:
                getattr(block, e)(make(e))


D = 1024
NT = 1024
TT = 512
NTT = NT // TT
DFF = 2816
NJ = DFF // 128
ALPHA = 4.0 ** 0.25
EPS = 1e-5


class WStream:
    def __init__(self, P, name, shape, nbuf=2, dtype=BF16, eng="gpsimd"):
        self.P, self.name, self.nbuf, self.eng = P, name, nbuf, eng
        self.bufs = [P.sb(shape, dtype, name=f"{name}_{i}") for i in range(nbuf)]
        self.i = 0

    def next(self):
        k = self.i % self.nbuf
        self.i += 1
        return k, self.bufs[k], f"{self.name}{k}"

    def load(self, k, dst_ap, src_ap, eng=None):
        key = f"{self.name}{k}"
        self.P.op(eng or self.eng, lambda e: e.dma_start(out=dst_ap, in_=src_ap), writes=[key], dma_key=key)


def build_B(nc, P, T, moe, half, tag="B"):
    E = 8 if moe else 1
    hs = slice(half * NT, (half + 1) * NT)
    P.push()
    P.prefix = f"{tag}h{half}_"
    PS = P.ps([128, 4096], F32, name="psB")
    bank = lambda i: PS[:, i * 512:(i + 1) * 512]
    bkey = lambda i: f"psB{i}"

    X32 = P.sb([128, 8, NT], F32, name="X32")
    Xbf = P.sb([128, 8, NT], BF16, name="Xbf")
    xk = lambda c, t: f"x32_{c}_{t}"
    xbk = lambda c, t: f"xbf_{c}_{t}"
    allx = [xk(c, t) for c in range(8) for t in range(NTT)]
    allxb = [xbk(c, t) for c in range(8) for t in range(NTT)]
    xT = T["x_in"].ap()[:, hs].rearrange("(c p) n -> p c n", p=128)
    wB = T["wB"].ap()
    for c in range(8):
        P.op("sync", lambda e, c=c: e.dma_start(out=X32[:, c, :], in_=xT[:, c, :]),
             reads=["out_x"], writes=[xk(c, t) for t in range(NTT)], dma_key=f"x32ld{c}")
        P.op("scalar", lambda e, c=c: e.copy(Xbf[:, c, :], X32[:, c, :]),
             reads=[xk(c, t) for t in range(NTT)], writes=[xbk(c, t) for t in range(NTT)])
    def small(name, shape, src, dtype=F32, eng="sync", grp="small"):
        t = P.sb(shape, dtype, name=name)
        if eng == "gpsimd":
            grp = grp + "_g"
        P.op(eng, lambda e: e.dma_start(out=t[:], in_=src), writes=[grp], dma_key=grp)
        return t

    ident = small("ident", [128, 128], T["ident"].ap())
    triU = small("triU", [128, 128], T["triU"].ap())
    bU = small("bU", [128, 6], T["bU"].ap())
    bG = small("bG", [128, 24], T["bG"].ap())
    lng = small("lng", [128, 16], T["lng"].ap())
    lnb = small("lnb", [128, 16], T["lnb"].ap())
    onesM = P.sb([128, 128], F32, name="onesM")
    P.op("vector", lambda e: e.memset(onesM[:], 1.0 / D), writes=["onesM"])
    epsc = P.sb([128, 1], F32, name="epsc")
    P.op("vector", lambda e: e.memset(epsc[:], EPS), writes=["epsc"])
    yb = P.sb([128, 6, NT], BF16, name="yb")
    P.push()
    Y8 = P.sb([128, 8, NT], BF16, name="Y8")
    P.push()
    wuv = T["wuv_buf"]
    bVb = small("bVb", [128, 768], T["bV"].ap().partition_broadcast(128))
    slg = small("slg", [128, 768], T["slg"].ap().partition_broadcast(128))
    slb = small("slb", [128, 768], T["slb"].ap().partition_broadcast(128))
    sgb = small("sgb", [1, 768], T["sgb"].ap(), dtype=BF16, eng="gpsimd")
    bV1 = small("bV1", [1, 768], T["bV"].ap(), dtype=BF16, eng="gpsimd")
    sgT32 = small("sgT32", [128, 6, 128], T["sgT"].ap().rearrange("g s t -> s g t"))
    ones_r = P.sb([1, 128], BF16, name="ones_r")
    P.op("vector", lambda e: e.memset(ones_r[:], 1.0), writes=["ones_r"])
    sgTb = P.sb([128, 6, 128], BF16, name="sgTb")
    for g in range(6):
        P.op("vector", lambda e, g=g: e.tensor_tensor(sgTb[:, g, :], sgT32[:, g, :], triU[:], ALU.mult),
             reads=["small"], writes=[f"sgTb{g}"])

    cand = [P.sb([128, 8, NT], BF16, name=f"ycand{i}") for i in range(2)]
    selh = small("selh", [128, 2], T["selh"].ap(), grp="small3")
    for i in range(2):
        yg = T["ygath"][i].ap()
        for q in range(8):
            r0 = q * 512 if q < 2 else ((q - 2) // 3) * 512 + 128 + ((q - 2) % 3) * 128
            P.op("sync", lambda e, i=i, q=q, r0=r0, yg=yg: e.dma_start(out=cand[i][:, q, :], in_=yg[r0:r0 + 128, hs]),
                 reads=[f"ygath{i}"], writes=[f"ycand{i}"], dma_key="yld")
    gv = [P.sb([128, 768], F32, name=f"gv{i}") for i in range(2)]
    vn = P.sb([128, 4, 768], BF16, name="vn")
    usb = P.sb([128, 6, TT], BF16, name="usb")
    stats = P.sb([128, 2, 6], F32, name="bnst")
    mv = P.sb([128, 2], F32, name="bnmv")
    rstd = P.sb([128, 1], F32, name="rstdv")
    ui = 0
    for tt in range(NTT):
        tsl = slice(tt * TT, (tt + 1) * TT)
        for g in range(6):
            b = ui % 2
            ui += 1
            for k in range(8):
                P.op("tensor", lambda e, b=b, k=k, g=g, tsl=tsl: e.matmul(bank(b), lhsT=wuv[:, k, g * 128:(g + 1) * 128], rhs=Xbf[:, k, tsl], start=(k == 0), stop=(k == 7)),
                     reads=[f"wUV{k}", xbk(k, tt)], writes=[bkey(b)])
            P.op("scalar", lambda e, b=b, g=g: e.activation(usb[:, g, :], bank(b), AF.Gelu_apprx_tanh, bias=bU[:, g:g + 1]),
                 reads=[bkey(b), "small"], writes=[f"usb{g}"])
        for cc in range(4):
            ch = tt * 4 + cc
            csl = slice(ch * 128, (ch + 1) * 128)
            b0 = 2 + 2 * (cc % 2)
            for k in range(8):
                P.op("tensor", lambda e, b0=b0, k=k, csl=csl: e.matmul(bank(b0), lhsT=Xbf[:, k, csl], rhs=wuv[:, k, 768:1280], start=(k == 0), stop=False),
                     reads=[f"wUV{k}", xbk(k, tt)], writes=[bkey(b0)])
            P.op("tensor", lambda e, b0=b0: e.matmul(bank(b0), lhsT=ones_r[0:1, :], rhs=bV1[0:1, 0:512], start=False, stop=True),
                 reads=["ones_r", "small_g"], writes=[bkey(b0)])
            for k in range(8):
                P.op("tensor", lambda e, b0=b0, k=k, csl=csl: e.matmul(PS[:, (b0 + 1) * 512:(b0 + 1) * 512 + 256], lhsT=Xbf[:, k, csl], rhs=wuv[:, k, 1280:1536], start=(k == 0), stop=False),
                     reads=[f"wUV{k}", xbk(k, tt)], writes=[bkey(b0 + 1)])
            P.op("tensor", lambda e, b0=b0: e.matmul(PS[:, (b0 + 1) * 512:(b0 + 1) * 512 + 256], lhsT=ones_r[0:1, :], rhs=bV1[0:1, 512:768], start=False, stop=True),
                 reads=["ones_r", "small_g"], writes=[bkey(b0 + 1)])
            g_ = gv[cc % 2]
            gk = f"gv{cc % 2}"
            P.op("scalar", lambda e, b0=b0, g_=g_: e.activation(g_[:, 0:512], bank(b0), AF.Gelu_apprx_tanh),
                 reads=[bkey(b0)], writes=[gk])
            P.op("scalar", lambda e, b0=b0, g_=g_: e.activation(g_[:, 512:768], PS[:, (b0 + 1) * 512:(b0 + 1) * 512 + 256], AF.Gelu_apprx_tanh),
                 reads=[bkey(b0 + 1)], writes=[gk])
            P.op("vector", lambda e, g_=g_: e.bn_stats(stats[:, 0, :], g_[:, 0:384]), reads=[gk], writes=["bnst0"])
            P.op("vector", lambda e, g_=g_: e.bn_stats(stats[:, 1, :], g_[:, 384:768]), reads=[gk], writes=["bnst1"])
            P.op("vector", lambda e: e.bn_aggr(mv[:], stats[:]), reads=["bnst0", "bnst1"], writes=["bnmv"])
            P.op("scalar", lambda e: e.activation(rstd[:], mv[:, 1:2], AF.Sqrt, bias=epsc[:]), reads=["bnmv", "epsc"], writes=["rstdv"])
            P.op("vector", lambda e: e.reciprocal(rstd[:], rstd[:]), reads=["rstdv"], writes=["rstdv"])
            P.op("vector", lambda e, g_=g_: e.tensor_scalar(g_[:], g_[:], mv[:, 0:1], rstd[:], ALU.subtract, ALU.mult),
                 reads=[gk, "bnmv", "rstdv"], writes=[gk])
            P.op("gpsimd", lambda e, g_=g_: e.tensor_tensor(g_[:], g_[:], slg[:], ALU.mult), reads=[gk, "small"], writes=[gk])
            P.op("gpsimd", lambda e, g_=g_, cc=cc: e.tensor_tensor(vn[:, cc, :], g_[:], slb[:], ALU.add), reads=[gk, "small"], writes=[f"vn{cc}"])
        for g in range(6):
            b = 6 + g % 2
            for cc in range(4):
                osl = slice(cc * 128, (cc + 1) * 128)
                P.op("tensor", lambda e, b=b, g=g, cc=cc, osl=osl: e.matmul(PS[:, b * 512 + cc * 128:b * 512 + (cc + 1) * 128], lhsT=vn[:, cc, g * 128:(g + 1) * 128], rhs=sgTb[:, g, :], start=True, stop=False),
                     reads=[f"vn{cc}", f"sgTb{g}"], writes=[bkey(b)])
                P.op("tensor", lambda e, b=b, g=g, cc=cc, osl=osl: e.matmul(PS[:, b * 512 + cc * 128:b * 512 + (cc + 1) * 128], lhsT=ones_r[0:1, :], rhs=sgb[0:1, g * 128:(g + 1) * 128], start=False, stop=True),
                     reads=["ones_r", "small_g"], writes=[bkey(b)])
            P.op("vector", lambda e, b=b, g=g, tsl=tsl: e.tensor_tensor(yb[:, g, tsl], bank(b), usb[:, g, :], ALU.mult),
                 reads=[bkey(b), f"usb{g}"], writes=[f"yb{g}_{tt}"])

    if T.get("after_S1_hook") is not None and half == 1:
        T["after_S1_hook"]()
    P.op("vector", lambda e: e.tensor_scalar(Y8[:], cand[0][:], selh[:, 0:1], None, ALU.mult), reads=["ycand0", "small3"], writes=["Y8"])
    P.op("vector", lambda e: e.scalar_tensor_tensor(Y8[:], cand[1][:], selh[:, 1:2], Y8[:], ALU.mult, ALU.add), reads=["ycand1", "small3", "Y8"], writes=["Y8"])
    P.pop()
    P.push()
    Z = P.sb([128, 8, NT], BF16, name="Z")
    wG = WStream(P, "wG", [128, 3, 8, 128], nbuf=2)
    wBR = WStream(P, "wBR", [128, 14, 128], nbuf=2)
    wbr = T["wbr"].ap()

    def load_z(m):
        kg, g_t, gkey = wG.next()
        for br in range(3):
            c0 = 1536 + br * 1024 + m * 128
            wG.load(kg, g_t[:, br, :, :], wB[:, c0:c0 + 128].rearrange("(k p) n -> p k n", p=128))
        kb, b_t, bkey_ = wBR.next()
        wBR.load(kb, b_t[:], wbr[:, :, m * 128:(m + 1) * 128].rearrange("q p n -> p q n"))
        return g_t, gkey, b_t, bkey_

    sig = [P.sb([128, TT], F32, name=f"sig{i}") for i in range(2)]
    zt = [P.sb([128, TT], F32, name=f"zt{i}") for i in range(2)]
    zacc = [P.sb([128, TT], F32, name=f"zacc{i}") for i in range(2)]
    nxt = load_z(0)
    pi = 0
    zi = 0
    brK = [[(0, 128, "Y8", lambda tsl, q=q: Y8[:, q, tsl]) for q in range(2)],
           [(0, 128, f"yb{g}", lambda tsl, g=g: yb[:, g, tsl]) for g in range(6)],
           [(0, 128, "Y8", lambda tsl, pc=pc: Y8[:, 2 + pc, tsl]) for pc in range(6)]]
    brOff = [0, 2, 8]
    wO = WStream(P, "wO", [128, 8, 1024], nbuf=1)
    ko, wo, wok = wO.next()
    for m in range(8):
        g_t, gkey, b_t, bkey_ = nxt
        if m + 1 < 8:
            nxt = load_z(m + 1)
        if m == 2:
            for k in range(8):
                wO.load(ko, wo[:, k, :], T["wo"].ap()[k * 128:(k + 1) * 128, :])
        for tt in range(NTT):
            tsl = slice(tt * TT, (tt + 1) * TT)
            za = zacc[zi % 2]
            zk = f"zacc{zi % 2}"
            zi += 1
            for br in range(3):
                bG_ = (2 * pi) % 8
                bP_ = bG_ + 1
                pi += 1
                for k in range(8):
                    P.op("tensor", lambda e, bG_=bG_, br=br, k=k, tsl=tsl, g_t=g_t: e.matmul(bank(bG_), lhsT=g_t[:, br, k, :], rhs=Xbf[:, k, tsl], start=(k == 0), stop=(k == 7)),
                         reads=[gkey, xbk(k, tt)], writes=[bkey(bG_)])
                pcs = brK[br]
                for qi, (p0, rows, rkey, apf) in enumerate(pcs):
                    rk = rkey if br != 1 else f"{rkey}_{tt}"
                    P.op("tensor", lambda e, bP_=bP_, qi=qi, rows=rows, apf=apf, tsl=tsl, br=br, b_t=b_t, n=len(pcs): e.matmul(bank(bP_), lhsT=b_t[0:rows, brOff[br] + qi, :], rhs=apf(tsl), start=(qi == 0), stop=(qi == n - 1)),
                         reads=[bkey_, rk], writes=[bkey(bP_)])
                s_ = sig[pi % 2]
                sk = f"sig{pi % 2}"
                P.op("scalar", lambda e, bG_=bG_, s_=s_, br=br, m=m: e.activation(s_[:], bank(bG_), AF.Sigmoid, bias=bG[:, br * 8 + m:br * 8 + m + 1]),
                     reads=[bkey(bG_), "small"], writes=[sk])
                if br == 0:
                    P.op("vector", lambda e, s_=s_, bP_=bP_, za=za: e.tensor_tensor(za[:], s_[:], bank(bP_), ALU.mult),
                         reads=[sk, bkey(bP_)], writes=[zk])
                else:
                    z_ = zt[pi % 2]
                    ztk = f"zt{pi % 2}"
                    P.op("vector", lambda e, s_=s_, bP_=bP_, z_=z_: e.tensor_tensor(z_[:], s_[:], bank(bP_), ALU.mult),
                         reads=[sk, bkey(bP_)], writes=[ztk])
                    if br == 1:
                        P.op("vector", lambda e, z_=z_, za=za: e.tensor_tensor(za[:], za[:], z_[:], ALU.add),
                             reads=[ztk, zk], writes=[zk])
                    else:
                        P.op("vector", lambda e, z_=z_, za=za, m=m, tsl=tsl: e.tensor_tensor(Z[:, m, tsl], za[:], z_[:], ALU.add),
                             reads=[ztk, zk], writes=[f"Z{m}_{tt}"])

    bi = 0
    for tt in range(NTT):
        tsl = slice(tt * TT, (tt + 1) * TT)
        for m in range(8):
            b = bi % 8
            bi += 1
            for k in range(8):
                P.op("tensor", lambda e, b=b, k=k, m=m, tsl=tsl: e.matmul(bank(b), lhsT=wo[:, k, m * 128:(m + 1) * 128], rhs=Z[:, k, tsl], start=(k == 0), stop=(k == 7)),
                     reads=[wok, f"Z{k}_{tt}"], writes=[bkey(b)])
            P.op("vector", lambda e, b=b, m=m, tsl=tsl: e.scalar_tensor_tensor(X32[:, m, tsl], X32[:, m, tsl], ALPHA, bank(b), ALU.mult, ALU.add),
                 reads=[bkey(b), xk(m, tt)], writes=[xk(m, tt)])

    P.pop()
    P.pop()
    sq = [P.sb([128, TT], F32, name=f"sq{i}") for i in range(2)]
    mean_sb = [P.sb([128, TT], F32, name=f"mean_sb{i}") for i in range(NTT)]
    rstd_sb = [P.sb([128, TT], F32, name=f"rstd_sb{i}") for i in range(NTT)]
    lt = [P.sb([128, TT], F32, name=f"lt{i}") for i in range(2)]

    def layer_norm(which, write_bf=True, bbase=0):
        for tt in range(NTT):
            tsl = slice(tt * TT, (tt + 1) * TT)
            b1, b2 = bbase + 2 * (tt % 2), bbase + 2 * (tt % 2) + 1
            for c in range(8):
                P.op("tensor", lambda e, c=c, tsl=tsl, b1=b1: e.matmul(bank(b1), lhsT=onesM[:], rhs=X32[:, c, tsl], start=(c == 0), stop=(c == 7)),
                     reads=["onesM", xk(c, tt)], writes=[bkey(b1)])
            for c in range(8):
                s_ = sq[c % 2]
                P.op("scalar", lambda e, c=c, tsl=tsl, s_=s_: e.activation(s_[:], X32[:, c, tsl], AF.Square),
                     reads=[xk(c, tt)], writes=[f"sq{c % 2}"])
                P.op("tensor", lambda e, c=c, s_=s_, b2=b2: e.matmul(bank(b2), lhsT=onesM[:], rhs=s_[:], start=(c == 0), stop=(c == 7)),
                     reads=["onesM", f"sq{c % 2}"], writes=[bkey(b2)])
        for tt in range(NTT):
            b1, b2 = bbase + 2 * (tt % 2), bbase + 2 * (tt % 2) + 1
            m_, r_ = mean_sb[tt], rstd_sb[tt]
            mk_, rk_ = f"mean_sb{tt}", f"rstd_sb{tt}"
            P.op("scalar", lambda e, b1=b1, m_=m_: e.copy(m_[:], bank(b1)), reads=[bkey(b1)], writes=[mk_])
            P.op("vector", lambda e, m_=m_, r_=r_: e.tensor_tensor(r_[:], m_[:], m_[:], ALU.mult), reads=[mk_], writes=[rk_])
            P.op("vector", lambda e, b2=b2, r_=r_: e.tensor_tensor(r_[:], bank(b2), r_[:], ALU.subtract), reads=[bkey(b2), rk_], writes=[rk_])
            P.op("scalar", lambda e, r_=r_: e.activation(r_[:], r_[:], AF.Sqrt, bias=epsc[:]), reads=[rk_, "epsc"], writes=[rk_])
            P.op("vector", lambda e, r_=r_: e.reciprocal(r_[:], r_[:]), reads=[rk_], writes=[rk_])
        li = 0
        for tt in range(NTT):
            tsl = slice(tt * TT, (tt + 1) * TT)
            m_, r_ = mean_sb[tt], rstd_sb[tt]
            mk_, rk_ = f"mean_sb{tt}", f"rstd_sb{tt}"
            for c in range(8):
                l_ = lt[li % 2]
                lk = f"lt{li % 2}"
                li += 1
                P.op("vector", lambda e, c=c, tsl=tsl, l_=l_, m_=m_: e.tensor_tensor(l_[:], X32[:, c, tsl], m_[:], ALU.subtract),
                     reads=[xk(c, tt), mk_], writes=[lk])
                P.op("gpsimd", lambda e, l_=l_, r_=r_: e.tensor_tensor(l_[:], l_[:], r_[:], ALU.mult),
                     reads=[lk, rk_], writes=[lk])
                col = which * 8 + c
                P.op("vector", lambda e, c=c, tsl=tsl, l_=l_, col=col: e.tensor_scalar(X32[:, c, tsl], l_[:], lng[:, col:col + 1], lnb[:, col:col + 1], ALU.mult, ALU.add),
                     reads=[lk, "small"], writes=[xk(c, tt)])
                if write_bf:
                    P.op("scalar", lambda e, c=c, tsl=tsl: e.copy(Xbf[:, c, tsl], X32[:, c, tsl]),
                         reads=[xk(c, tt)], writes=[xbk(c, tt)])

    layer_norm(0)

    P.push()
    if moe:
        rw = small("rw", [128, 8, 8], T["rw"].ap().rearrange("(k p) e -> p k e", p=128), grp="small2")
        rb1 = small("rb1", [1, 8], T["rb"].ap(), grp="small2")
        sel8 = small("sel8", [8, 8, 128], T["sel8"].ap().rearrange("k (e m) -> k e m", m=128), grp="small2")
        ones32 = P.sb([1, 128], F32, name="ones32")
        P.op("vector", lambda e: e.memset(ones32[:], 1.0), writes=["ones32"])
        Lg = P.sb([128, NT // 128, 8], F32, name="Lg")
        gate = P.sb([128, NT // 128, 8], F32, name="gate")
        mx8 = P.sb([128, 8], F32, name="mx8")
        nm1 = P.sb([128, 1], F32, name="nm1")
        selm = P.sb([128, 8], F32, name="selm")
        ex = P.sb([128, 8], F32, name="ex")
        den = P.sb([128, 1], F32, name="den")
        gateT = P.sb([8, NT], F32, name="gateT")
        for ch in range(NT // 128):
            csl = slice(ch * 128, (ch + 1) * 128)
            tt = ch // 4
            for k in range(8):
                P.op("tensor", lambda e, k=k, csl=csl, ch=ch: e.matmul(PS[:, ch * 8:(ch + 1) * 8], lhsT=X32[:, k, csl], rhs=rw[:, k, :], start=(k == 0), stop=False),
                     reads=["small2", xk(k, tt)], writes=[bkey(0)])
            P.op("tensor", lambda e, ch=ch: e.matmul(PS[:, ch * 8:(ch + 1) * 8], lhsT=ones32[0:1, :], rhs=rb1[0:1, :], start=False, stop=True),
                 reads=["small2", "ones32"], writes=[bkey(0)])
        P.op("vector", lambda e: e.tensor_copy(Lg[:].rearrange("p c e -> p (c e)"), PS[:, 0:NT // 16]),
             reads=[bkey(0)], writes=["Lg"])
        for ch in range(NT // 128):
            P.op("vector", lambda e, ch=ch: e.max(mx8[:], Lg[:, ch, :]), reads=["Lg"], writes=["mx8"])
            P.op("vector", lambda e: e.tensor_scalar(nm1[:], mx8[:, 0:1], -1.0, None, ALU.mult), reads=["mx8"], writes=["nm1"])
            P.op("vector", lambda e, ch=ch: e.tensor_scalar(selm[:], Lg[:, ch, :], mx8[:, 1:2], None, ALU.is_ge), reads=["Lg", "mx8"], writes=["selm"])
            P.op("scalar", lambda e, ch=ch: e.activation(ex[:], Lg[:, ch, :], AF.Exp, bias=nm1[:]), reads=["Lg", "nm1"], writes=["ex"])
            P.op("vector", lambda e: e.tensor_tensor(ex[:], ex[:], selm[:], ALU.mult), reads=["ex", "selm"], writes=["ex"])
            P.op("vector", lambda e: e.reduce_sum(den[:], ex[:], axis=AX.X), reads=["ex"], writes=["den"])
            P.op("vector", lambda e: e.reciprocal(den[:], den[:]), reads=["den"], writes=["den"])
            P.op("vector", lambda e, ch=ch: e.tensor_scalar(gate[:, ch, :], ex[:], den[:], None, ALU.mult), reads=["ex", "den"], writes=[f"gate{ch}"])
            P.op("tensor", lambda e, ch=ch: e.transpose(PS[0:8, (1 + ch // 4) * 512 + (ch % 4) * 128:(1 + ch // 4) * 512 + (ch % 4 + 1) * 128], gate[:, ch, :], ident[:]),
                 reads=[f"gate{ch}", "small"], writes=[bkey(1 + ch // 4)])
        for tt in range(NTT):
            P.op("vector", lambda e, tt=tt: e.tensor_copy(gateT[:, tt * TT:(tt + 1) * TT], PS[0:8, (1 + tt) * 512:(2 + tt) * 512]),
                 reads=[bkey(1 + tt)], writes=[f"gateT{tt}"])
        gbc = P.sb([128, 2 * TT], F32, name="gbc")

    G = NT
    act = P.sb([128, NJ, G], BF16, name="act")
    w13 = WStream(P, "w13", [128, 2, 8, 256], nbuf=2)
    w2s = WStream(P, "w2s", [128, NJ, 128], nbuf=2)
    sil = [P.sb([128, TT], F32, name=f"sil{i}") for i in range(2)]
    ftmp = [P.sb([128, TT], F32, name=f"ftmp{i}") for i in range(2)]
    w1d, w3d, w2d = T["w1"].ap(), T["w3"].ap(), T["w2"].ap()

    def load13(e_, jj):
        k_, t_, key_ = w13.next()
        w13.load(k_, t_[:, 0, :, :], w1d[e_, :, jj * 256:(jj + 1) * 256].rearrange("(k p) n -> p k n", p=128))
        w13.load(k_, t_[:, 1, :, :], w3d[e_, :, jj * 256:(jj + 1) * 256].rearrange("(k p) n -> p k n", p=128))
        return t_, key_

    def load2(e_, m):
        k_, t_, key_ = w2s.next()
        w2s.load(k_, t_[:], w2d[e_, :, m * 128:(m + 1) * 128].rearrange("(j p) n -> p j n", p=128))
        return t_, key_

    hb = 0
    si = 0
    first_scale_done = set()
    for tg in range(1):
        for e_ in range(E):
            if moe:
                for h in range(2):
                    tt = tg * 2 + h
                    P.op("tensor", lambda e, e_=e_, tt=tt, h=h: e.matmul(bank(6 + h), lhsT=sel8[:, e_, :], rhs=gateT[:, tt * TT:(tt + 1) * TT], start=True, stop=True),
                         reads=["small2", f"gateT{tt}"], writes=[bkey(6 + h)])
                    P.op("scalar", lambda e, h=h: e.copy(gbc[:, h * TT:(h + 1) * TT], bank(6 + h)), reads=[bkey(6 + h)], writes=[f"gbc{h}"])
            nx13 = load13(e_, 0)
            for jj in range(NJ // 2):
                w_t, wkey = nx13
                if jj + 1 < NJ // 2:
                    nx13 = load13(e_, jj + 1)
                for jh in range(2):
                    j = jj * 2 + jh
                    for h in range(2):
                        tt = tg * 2 + h
                        tsl = slice(tt * TT, (tt + 1) * TT)
                        b1 = (hb % 3) * 2
                        b3 = b1 + 1
                        hb += 1
                        for k in range(8):
                            P.op("tensor", lambda e, b1=b1, k=k, jh=jh, tsl=tsl, w_t=w_t: e.matmul(bank(b1), lhsT=w_t[:, 0, k, jh * 128:(jh + 1) * 128], rhs=Xbf[:, k, tsl], start=(k == 0), stop=(k == 7)),
                                 reads=[wkey, xbk(k, tt)], writes=[bkey(b1)])
                        for k in range(8):
                            P.op("tensor", lambda e, b3=b3, k=k, jh=jh, tsl=tsl, w_t=w_t: e.matmul(bank(b3), lhsT=w_t[:, 1, k, jh * 128:(jh + 1) * 128], rhs=Xbf[:, k, tsl], start=(k == 0), stop=(k == 7)),
                                 reads=[wkey, xbk(k, tt)], writes=[bkey(b3)])
                        s_ = sil[si % 2]
                        sk = f"sil{si % 2}"
                        si += 1
                        P.op("scalar", lambda e, b1=b1, s_=s_: e.activation(s_[:], bank(b1), AF.Silu), reads=[bkey(b1)], writes=[sk])
                        P.op("vector", lambda e, b3=b3, s_=s_, j=j, h=h: e.tensor_tensor(act[:, j, h * TT:(h + 1) * TT], s_[:], bank(b3), ALU.mult),
                             reads=[sk, bkey(b3)], writes=[f"act{j}_{h}"])
            nx2 = load2(e_, 0)
            for m in range(8):
                w_t, wkey = nx2
                if m + 1 < 8:
                    nx2 = load2(e_, m + 1)
                for h in range(2):
                    tt = tg * 2 + h
                    tsl = slice(tt * TT, (tt + 1) * TT)
                    b = (hb % 3) * 2
                    hb += 1
                    for j in range(NJ):
                        P.op("tensor", lambda e, b=b, j=j, h=h, w_t=w_t: e.matmul(bank(b), lhsT=w_t[:, j, :], rhs=act[:, j, h * TT:(h + 1) * TT], start=(j == 0), stop=(j == NJ - 1)),
                             reads=[wkey, f"act{j}_{h}"], writes=[bkey(b)])
                    if not moe:
                        P.op("vector", lambda e, b=b, m=m, tsl=tsl: e.scalar_tensor_tensor(X32[:, m, tsl], X32[:, m, tsl], ALPHA, bank(b), ALU.mult, ALU.add),
                             reads=[bkey(b), xk(m, tt)], writes=[xk(m, tt)])
                    else:
                        f_ = ftmp[si % 2]
                        fk = f"ftmp{si % 2}"
                        si += 1
                        P.op("vector", lambda e, b=b, f_=f_, h=h: e.tensor_tensor(f_[:], bank(b), gbc[:, h * TT:(h + 1) * TT], ALU.mult),
                             reads=[bkey(b), f"gbc{h}"], writes=[fk])
                        if (m, tt) not in first_scale_done:
                            first_scale_done.add((m, tt))
                            P.op("vector", lambda e, f_=f_, m=m, tsl=tsl: e.scalar_tensor_tensor(X32[:, m, tsl], X32[:, m, tsl], ALPHA, f_[:], ALU.mult, ALU.add),
                                 reads=[fk, xk(m, tt)], writes=[xk(m, tt)])
                        else:
                            P.op("vector", lambda e, f_=f_, m=m, tsl=tsl: e.tensor_tensor(X32[:, m, tsl], X32[:, m, tsl], f_[:], ALU.add),
                                 reads=[fk, xk(m, tt)], writes=[xk(m, tt)])

    P.pop()
    layer_norm(1, write_bf=(T.get("xob") is not None), bbase=4)

    xo = T["xo"].ap()[:, hs].rearrange("(c p) n -> p c n", p=128)
    for c in range(8):
        P.op("sync", lambda e, c=c: e.dma_start(out=xo[:, c, :], in_=X32[:, c, :]), reads=[xk(c, t) for t in range(NTT)], writes=["out_x"], dma_key="outx")
        if T.get("xob") is not None:
            xb_t = T["xob"][c // 4].ap()
            P.op("sync", lambda e, c=c, xb_t=xb_t: e.dma_start(out=xb_t[(c % 4) * 128:(c % 4 + 1) * 128, hs], in_=Xbf[:, c, :]), reads=[xbk(c, t) for t in range(NTT)], writes=["out_xb"], dma_key="outx")
    P.wait_res("sync", ["out_x", "out_xb"])
    P.pop()


SUB = 9

S = 4096
TT = 512
NTA = S // TT
DILS = (1, 4, 16)
NA = 2692
LNSC = math.log(192 ** -0.5)
TWO_PI = 2.0 * math.pi
CW1 = 6.28125
CW2 = TWO_PI - CW1
MAGIC = 12582912.0


def build_A(nc, P, T, tag="A", stage=99):
    P.push()
    P.prefix = f"{tag}_"
    PS = P.ps([128, 4096], F32, name="psA")
    bank = lambda i: PS[:, i * 512:(i + 1) * 512]
    bkey = lambda i: f"psA{i}"

    def small(name, shape, src, dtype=F32, eng="sync", grp="smallA"):
        t = P.sb(shape, dtype, name=name)
        if eng == "gpsimd":
            grp = grp + "_g"
        P.op(eng, lambda e: e.dma_start(out=t[:], in_=src), writes=[grp], dma_key=grp)
        return t

    xTa = P.sb([128, 8, S], BF16, name="xTa")
    if T["xsrc"][0] == "ext":
        xsrc = T["xsrc"][1].ap().rearrange("(c p) n -> p c n", p=128)
        for tt in range(NTA):
            tsl = slice(tt * TT, (tt + 1) * TT)
            P.op("gpsimd", lambda e, tsl=tsl: e.dma_start(out=xTa[:, :, tsl], in_=xsrc[:, :, tsl]), writes=[f"xTa{tt}"], dma_key=f"xTa{tt % 4}")
    else:
        for r in range(2):
            for hq in range(4):
                tt = r * 4 + hq
                for i in range(2):
                    xg = T["xsrc"][1 + i].ap()
                    P.op("sync", lambda e, r=r, hq=hq, i=i, xg=xg, tt=tt: e.dma_start(out=xTa[:, i * 4:(i + 1) * 4, tt * TT:(tt + 1) * TT], in_=xg[r * 512:(r + 1) * 512, hq * TT:(hq + 1) * TT].rearrange("(c p) n -> p c n", p=128)),
                         reads=["xg"], writes=[f"xTa{tt}"], dma_key=f"xTa_h{tt % 4}")

    ident = small("ident", [128, 128], T["ident"].ap())
    triU = small("triU", [128, 128], T["triU"].ap())
    permM = small("permM", [128, 128], T["permM"].ap())
    invf = small("invf", [128, 1], T["invf"].ap())
    sgn = small("sgn", [128, 1], T["sgn"].ap())
    maskA = small("maskA", [128, 512], T["maskA"].ap(), dtype=BF16, eng="gpsimd")
    bqk = small("bqk", [128, 6], T["bqk"].ap())
    bva = small("bva", [128, 384], T["bva"].ap().partition_broadcast(128))
    bml = small("bml", [128, 8], T["bml"].ap())
    bvm = small("bvm", [128, 384], T["bvm"].ap().partition_broadcast(128))
    bif = small("bif", [128, 4], T["bif"].ap().partition_broadcast(128))
    cw = small("cw", [128, 8, 4], T["cw"].ap())
    cb = small("cb", [128, 8], T["cb"].ap())
    bo = small("bo", [128, 6], T["bo"].ap())
    ones_bf = P.sb([128, 128], BF16, name="ones_bf")
    P.op("vector", lambda e: e.memset(ones_bf[:], 1.0), writes=["ones_bf"])
    ones_f = P.sb([128, 128], F32, name="ones_f")
    P.op("vector", lambda e: e.memset(ones_f[:], 1.0), writes=["ones_f"])
    lnsc = P.sb([128, 1], F32, name="lnsc")
    P.op("vector", lambda e: e.memset(lnsc[:], LNSC), writes=["lnsc"])

    wA = T["wA"].ap()

    P.push()
    cosT = P.sb([128, S], F32, name="cosT")
    sinT = P.sb([128, S], F32, name="sinT")
    P.push()
    posi = P.sb([128, S], I32, name="posi")
    P.op("sync", lambda e: e.dma_start(out=posi[:], in_=T["pos"].ap().partition_broadcast(128)), writes=["posi"], dma_key="posi")
    ang = P.sb([128, S], F32, name="ang")
    kk = P.sb([128, S], F32, name="kk")
    rr = P.sb([128, S], F32, name="rr")
    P.op("vector", lambda e: e.tensor_copy(ang[:], posi[:]), reads=["posi"], writes=["ang"])
    P.op("vector", lambda e: e.tensor_scalar(ang[:], ang[:], invf[:], None, ALU.mult), reads=["ang", "smallA"], writes=["ang"])
    P.op("vector", lambda e: e.tensor_scalar(kk[:], ang[:], 1.0 / TWO_PI, MAGIC, ALU.mult, ALU.add), reads=["ang"], writes=["kk"])
    P.op("vector", lambda e: e.tensor_scalar(kk[:], kk[:], MAGIC, None, ALU.subtract), reads=["kk"], writes=["kk"])
    P.op("vector", lambda e: e.scalar_tensor_tensor(rr[:], kk[:], -CW1, ang[:], ALU.mult, ALU.add), reads=["kk", "ang"], writes=["rr"])
    P.op("vector", lambda e: e.scalar_tensor_tensor(rr[:], kk[:], -CW2, rr[:], ALU.mult, ALU.add), reads=["kk", "rr"], writes=["rr"])
    P.op("vector", lambda e: e.tensor_scalar(rr[:], rr[:], math.pi, -math.pi, ALU.min, ALU.max), reads=["rr"], writes=["rr"])
    P.op("scalar", lambda e: e.activation(sinT[:], rr[:], AF.Sin, scale=sgn[:]), reads=["rr", "smallA"], writes=["sinT"])
    P.op("vector", lambda e: e.tensor_scalar(ang[:], rr[:], math.pi / 2, None, ALU.add), reads=["rr"], writes=["ang"])
    P.op("vector", lambda e: e.tensor_scalar(kk[:], ang[:], math.pi, None, ALU.is_gt), reads=["ang"], writes=["kk"])
    P.op("vector", lambda e: e.scalar_tensor_tensor(ang[:], kk[:], -TWO_PI, ang[:], ALU.mult, ALU.add), reads=["kk", "ang"], writes=["ang"])
    P.op("vector", lambda e: e.tensor_scalar(ang[:], ang[:], math.pi, -math.pi, ALU.min, ALU.max), reads=["ang"], writes=["ang"])
    P.op("scalar", lambda e: e.activation(cosT[:], ang[:], AF.Sin), reads=["ang"], writes=["cosT"])
    P.pop(dma=False)
    if stage == 1:
        P.op("sync", lambda e: e.dma_start(out=T["dbg"].ap()[:, 0:S], in_=cosT[:]), reads=["cosT"], writes=["dbgo"], dma_key="dbg")
        P.op("sync", lambda e: e.dma_start(out=T["dbg"].ap()[:, S:2 * S], in_=sinT[:]), reads=["sinT"], writes=["dbgo"], dma_key="dbg")
        P.wait_res("sync", ["dbgo"])
        P.pop(); P.pop()
        return

    Y = P.sb([128, 2, S], F32, name="Y")
    QK = [P.sb([128, 2, S], BF16, name=f"QK{i}") for i in range(2)]
    V = [P.sb([128, 32, 128], BF16, name=f"V{i}") for i in range(2)]
    wAt = WStream(P, "wAt", [128, 8, 384], nbuf=2)
    qf = [P.sb([128, TT], F32, name=f"qf{i}") for i in range(2)]
    t1 = [P.sb([128, TT], F32, name=f"t1{i}") for i in range(1)] * 2
    t2 = [P.sb([128, TT], F32, name=f"t2{i}") for i in range(1)] * 2
    pT = [P.sb([128, 512], BF16, name=f"pT{i}") for i in range(2)]
    cnt_a = {"ci": 0}
    B_PROJ, B_SW, B_V, B_PV = 0, 1, 1, (2, 3)

    def make_group(g, dil):
        gp = g % 2
        kq, w_t, wkey = wAt.next()
        QKg, Vg = QK[gp], V[gp]
        qkk = lambda which, tt: f"QK{gp}_{which}_{tt}"
        span = 128 * dil
        blocks = [(n, r) for n in range(S // span) for r in range(dil)]
        tok = lambda n, r: slice(n * span + r, n * span + r + span - dil + 1, dil)
        tiles_of = lambda n, r: sorted(set(range((n * span) // TT, (n * span + span - 1) // TT + 1)))
        units = []

        def load_w():
            for k in range(8):
                wAt.load(kq, w_t[:, k, :], wA[k * 128:(k + 1) * 128, g * 384:(g + 1) * 384])

        def qk_unit(tt, which):
            tsl = slice(tt * TT, (tt + 1) * TT)
            ci = cnt_a["ci"]
            cnt_a["ci"] += 1
            q_, qk_ = qf[ci % 2], f"qf{ci % 2}"
            a_, ak_ = t1[0], "t1_0"
            b_, bk_ = t2[0], "t2_0"
            for k in range(8):
                P.op("tensor", lambda e, k=k: e.matmul(bank(B_PROJ), lhsT=w_t[:, k, which * 128:(which + 1) * 128], rhs=xTa[:, k, tsl], start=(k == 0), stop=(k == 7)),
                     reads=[wkey, f"xTa{tt}"], writes=[bkey(B_PROJ)])
            col = 2 * g + which
            P.op("scalar", lambda e: e.activation(q_[:], bank(B_PROJ), AF.Identity, bias=bqk[:, col:col + 1]),
                 reads=[bkey(B_PROJ), "smallA"], writes=[qk_])
            P.op("tensor", lambda e: e.matmul(bank(B_SW), lhsT=permM[:], rhs=q_[:], start=True, stop=True),
                 reads=[qk_, "smallA"], writes=[bkey(B_SW)])
            P.op("vector", lambda e: e.tensor_tensor(a_[:], q_[:], cosT[:, tsl], ALU.mult), reads=[qk_, "cosT"], writes=[ak_])
            P.op("vector", lambda e: e.tensor_tensor(b_[:], bank(B_SW), sinT[:, tsl], ALU.mult), reads=[bkey(B_SW), "sinT"], writes=[bk_])
            P.op("vector", lambda e: e.tensor_tensor(QKg[:, which, tsl], a_[:], b_[:], ALU.add), reads=[ak_, bk_], writes=[qkk(which, tt)])

        def v_unit(bi4):
            for q4 in range(4):
                n, r = blocks[bi4 + q4]
                for k in range(8):
                    P.op("tensor", lambda e, k=k, q4=q4, tk=tok(n, r): e.matmul(PS[:, B_V * 512 + q4 * 128:B_V * 512 + (q4 + 1) * 128], lhsT=xTa[:, k, tk], rhs=w_t[:, k, 256:384], start=(k == 0), stop=(k == 7)),
                         reads=[wkey] + [f"xTa{t_}" for t_ in tiles_of(n, r)], writes=[bkey(B_V)])
            for q4 in range(4):
                P.op("vector", lambda e, q4=q4: e.tensor_tensor(Vg[:, bi4 + q4, :], PS[:, B_V * 512 + q4 * 128:B_V * 512 + (q4 + 1) * 128], bva[:, g * 128:(g + 1) * 128], ALU.add),
                     reads=[bkey(B_V), "smallA"], writes=[f"V{gp}_{bi4 + q4}"])

        units.append(load_w)
        qk_units = [(lambda tt=tt, which=which: qk_unit(tt, which)) for tt in range(NTA) for which in range(2)]
        v_units = [(lambda bi4=bi4: v_unit(bi4)) for bi4 in range(0, 32, 4)]
        if g == 0:
            units += v_units + qk_units
        else:
            units += qk_units + v_units

        def blk_setup(bi, n, r):
            bs = 4 + 2 * (bi % 2)
            bp = B_PV[bi % 2]
            p_ = pT[bi % 2]
            pk_ = f"pT{bi % 2}"
            kbs = [(n, 0)] + ([(n - 1, 1)] if n >= 1 else [])
            ncol = 128 * len(kbs)
            qreads = [qkk(0, t_) for t_ in tiles_of(n, r)]
            return bs, bp, p_, pk_, kbs, ncol, qreads

        def front(bi):
            n, r = blocks[bi]
            bs, bp, p_, pk_, kbs, ncol, qreads = blk_setup(bi, n, r)
            for (kn, slot) in kbs:
                kreads = [qkk(1, t_) for t_ in tiles_of(kn, r)]
                for hd in range(2):
                    c0 = (bs + hd) * 512 + slot * 128
                    P.op("tensor", lambda e, c0=c0, hd=hd, tkk=tok(kn, r), tkq=tok(n, r): e.matmul(PS[:, c0:c0 + 128], lhsT=QKg[hd * 64:(hd + 1) * 64, 1, tkk], rhs=QKg[hd * 64:(hd + 1) * 64, 0, tkq], start=True, stop=True),
                         reads=qreads + kreads, writes=[bkey(bs + hd)])
            for hd in range(2):
                P.op("scalar", lambda e, hd=hd: e.activation(p_[:, hd * 256:hd * 256 + ncol], PS[:, (bs + hd) * 512:(bs + hd) * 512 + ncol], AF.Exp, scale=0.125),
                     reads=[bkey(bs + hd)], writes=[pk_ + f"h{hd}"])
                P.op("vector", lambda e, hd=hd: e.tensor_tensor(p_[:, hd * 256:hd * 256 + ncol], p_[:, hd * 256:hd * 256 + ncol], maskA[:, 0:ncol], ALU.mult),
                     reads=[pk_ + f"h{hd}", "smallA_g"], writes=[pk_ + f"h{hd}"])

        def back(bi):
            n, r = blocks[bi]
            bs, bp, p_, pk_, kbs, ncol, qreads = blk_setup(bi, n, r)
            for hd in range(2):
                for oi in range(2):
                    c0 = bp * 512 + hd * 256 + oi * 128
                    for ki, (kn, slot) in enumerate(kbs):
                        kblk = blocks.index((kn, r))
                        rhs_c = hd * 256 + slot * 128
                        if oi == 0:
                            P.op("tensor", lambda e, c0=c0, kblk=kblk, rhs_c=rhs_c, ki=ki, nk=len(kbs): e.matmul(PS[:, c0:c0 + 128], lhsT=Vg[:, kblk, :], rhs=p_[:, rhs_c:rhs_c + 128], start=(ki == 0), stop=(ki == nk - 1)),
                                 reads=[pk_ + f"h{hd}", f"V{gp}_{kblk}"], writes=[bkey(bp)])
                        else:
                            P.op("tensor", lambda e, c0=c0, rhs_c=rhs_c, ki=ki, nk=len(kbs): e.matmul(PS[:, c0:c0 + 128], lhsT=ones_bf[:], rhs=p_[:, rhs_c:rhs_c + 128], start=(ki == 0), stop=(ki == nk - 1)),
                                 reads=[pk_ + f"h{hd}", "ones_bf"], writes=[bkey(bp)])
            for hd in range(2):
                rs = slice(hd * 64, (hd + 1) * 64)
                src = PS[rs, bp * 512 + hd * 256:bp * 512 + hd * 256 + 256].rearrange("p (a q) -> p a q", a=2)
                ykeys = [f"Y{hd}_{t_}" for t_ in tiles_of(n, r)]
                if g == 0:
                    P.op("vector", lambda e, rs=rs, src=src, tkq=tok(n, r): e.tensor_copy(Y[rs, :, tkq], src),
                         reads=[bkey(bp)], writes=ykeys)
                else:
                    P.op("vector", lambda e, rs=rs, src=src, tkq=tok(n, r): e.tensor_tensor(Y[rs, :, tkq], Y[rs, :, tkq], src, ALU.add),
                         reads=[bkey(bp)] + ykeys, writes=ykeys)

        return units, len(blocks), front, back

    groups = [make_group(g, dil) for g, dil in enumerate(DILS)]
    for u in groups[0][0]:
        u()
    for g in range(len(DILS)):
        units, nblk, front, back = groups[g]
        nxt_units = list(groups[g + 1][0]) if g + 1 < len(DILS) else []
        front(0)
        for bi in range(nblk):
            if bi + 1 < nblk:
                front(bi + 1)
            back(bi)
            if nxt_units:
                nxt_units.pop(0)()
        for u in nxt_units:
            u()

    yao = [P.sb([128, TT], BF16, name=f"yao{i}") for i in range(2)]
    for tt in (range(NTA) if stage not in (20, 21, 24, 25) else []):
        tsl = slice(tt * TT, (tt + 1) * TT)
        yt_ = yao[tt % 2]
        P.op("vector", lambda e, tsl=tsl: e.reciprocal(Y[:, 1, tsl], Y[:, 1, tsl]), reads=[f"Y0_{tt}", f"Y1_{tt}"], writes=[f"Yd_{tt}"])
        P.op("vector", lambda e, tsl=tsl, yt_=yt_: e.tensor_tensor(yt_[:], Y[:, 0, tsl], Y[:, 1, tsl], ALU.mult), reads=[f"Yd_{tt}", f"Y0_{tt}", f"Y1_{tt}"], writes=[f"yao{tt % 2}"])
        P.op("sync", lambda e, tt=tt, yt_=yt_: e.dma_start(out=T["ysend"][tt // 4].ap()[0:128, (tt % 4) * TT:(tt % 4 + 1) * TT], in_=yt_[:]), reads=[f"yao{tt % 2}"], writes=["out_ya"], dma_key="outA")
    P.wait_res("sync", ["out_ya"])
    P.pop()

    if stage == 2 or 20 <= stage <= 25:
        P.pop()
        return
    if T.get("mid_hook") is not None:
        T["mid_hook"]()
    P.push()
    PROJ, VIF, GB, OB, DB = 0, 1, (2, 3), (4, 5), (6, 7)
    wMl = WStream(P, "wMl", [128, 8, 1540], nbuf=1)
    km, wm, wmk = wMl.next()
    for k in range(8):
        wMl.load(km, wm[:, k, :], wA[k * 128:(k + 1) * 128, 1152:2692])
    PW = [128, 64, 128, 64]
    POFF = [0, 128, 192, 320]
    cbuf = P.sb([128, 8, 3 + TT], BF16, name="cbuf")
    P.op("vector", lambda e: e.memset(cbuf[:], 0.0), writes=[f"cbuf{i}" for i in range(8)])
    Dg = P.sb([128, 8, 4, 128], BF16, name="Dg")
    for pid_ in range(8):
        for j_ in range(4):
            P.op("vector", lambda e, pid_=pid_, j_=j_: e.tensor_scalar(Dg[:, pid_, j_, :], ident[:], cw[:, pid_, j_:j_ + 1], None, ALU.mult),
                 reads=["smallA"], writes=[f"Dg{pid_}"])
    QKm = [P.sb([128, 8, TT], BF16, name=f"QKm{i}") for i in range(2)]
    K32 = [P.sb([128, 4, TT], F32, name=f"K32{i}") for i in range(2)]
    og = [P.sb([128, 6, TT], BF16, name=f"og{i}") for i in range(2)]
    vaug = [P.sb([128, 4, 2, 256], BF16, name=f"vaug{i}") for i in range(2)]
    for i in range(2):
        P.op("vector", lambda e, i=i: e.memset(vaug[i][:, :, :, 192:256], 1.0), writes=[f"vaug{i}"])
    ifs = [P.sb([128, 4, 4], F32, name=f"ifs{i}") for i in range(2)]
    lpos = [P.sb([128, 4, 2], F32, name=f"lpos{i}") for i in range(2)]
    C32 = [[P.sb([128, 256], F32, name=f"C32_{h}{ab}") for ab in "AB"] for h in range(2)]
    Cb = [[P.sb([128, 256], BF16, name=f"Cb_{h}{ab}") for ab in "AB"] for h in range(2)]
    for h in range(2):
        for ab in range(2):
            P.op("vector", lambda e, h=h, ab=ab: e.memset(C32[h][ab][:], 0.0), writes=[f"C32_{h}{ab}"])
            P.op("vector", lambda e, h=h, ab=ab: e.memset(Cb[h][ab][:], 0.0), writes=[f"Cb_{h}{ab}"])
    NEGm = P.sb([128, 128], F32, name="NEGm")
    P.op("vector", lambda e: e.tensor_scalar(NEGm[:], triU[:], -1.0, 1.0e4, ALU.add, ALU.mult), reads=["smallA"], writes=["NEGm"])
    NEG2 = P.sb([128, 2, 128], F32, name="NEG2")
    for h in range(2):
        P.op("vector", lambda e, h=h: e.tensor_copy(NEG2[:, h, :], NEGm[:]), reads=["NEGm"], writes=["NEG2"])
    LFb2 = P.sb([128, 2, 128], F32, name="LFb2")
    acolT = P.sb([128, 4], F32, name="acolT")
    wpre2 = P.sb([128, 2], F32, name="wpre2")
    DTa2 = P.sb([128, 2, 128], F32, name="DTa2")
    DT2 = P.sb([128, 2, 128], F32, name="DT2")
    Erow2 = P.sb([128, 2, 128], F32, name="Erow2")
    dbl = lambda nm, shape, dt: [P.sb(shape, dt, name=f"{nm}_{c2}") for c2 in range(2)]
    wcol2 = dbl("wcol2", [128, 2], F32)
    dec2 = dbl("dec2", [128, 2], F32)
    scT2 = dbl("scT2", [128, 2, 128], BF16)
    qsA2 = dbl("qsA2", [128, 2, 128], BF16)
    qsB2 = dbl("qsB2", [128, 2, 128], BF16)
    kw2 = dbl("kw2", [128, 2, 192], BF16)
    rden2 = P.sb([128, 2, 128], F32, name="rden2")
    ogr2 = P.sb([128, 3, 2, 128], F32, name="ogr2")
    yco2 = P.sb([128, 2, 3, TT], BF16, name="yco2")
    cnt = {"pi": 0}

    def proj_groups(tt):
        tsl = slice(tt * TT, (tt + 1) * TT)
        par = tt % 2
        QKt, K32t, ogt, vat, ift, lpt = QKm[par], K32[par], og[par], vaug[par], ifs[par], lpos[par]
        groups = []

        def qk_piece(which, pc):
            w_, o_ = PW[pc], which * 384 + POFF[pc]
            pid = which * 4 + pc
            cnt["pi"] += 1
            pb = (PROJ, VIF)[cnt["pi"] % 2]
            for k in range(8):
                P.op("tensor", lambda e, k=k: e.matmul(PS[0:w_, pb * 512:(pb + 1) * 512], lhsT=wm[:, k, o_:o_ + w_], rhs=xTa[:, k, tsl], start=(k == 0), stop=(k == 7)),
                     reads=[wmk, f"xTa{tt}"], writes=[bkey(pb)])
            ck = f"cbuf{pid}"
            P.op("vector", lambda e: e.tensor_copy(cbuf[0:w_, pid, 0:3], cbuf[0:w_, pid, TT:TT + 3]), reads=[ck], writes=[ck])
            P.op("scalar", lambda e: e.activation(cbuf[0:w_, pid, 3:3 + TT], PS[0:w_, pb * 512:(pb + 1) * 512], AF.Identity, bias=bml[0:w_, pid:pid + 1]),
                 reads=[bkey(pb), "smallA", ck], writes=[ck])
            for j in range(4):
                P.op("tensor", lambda e, j=j: e.matmul(PS[0:w_, pb * 512:(pb + 1) * 512], lhsT=Dg[0:w_, pid, j, 0:w_], rhs=cbuf[0:w_, pid, j:j + TT], start=(j == 0), stop=(j == 3)),
                     reads=[ck, f"Dg{pid}"], writes=[bkey(pb)])
            P.op("scalar", lambda e: e.activation(QKt[0:w_, pid, :], PS[0:w_, pb * 512:(pb + 1) * 512], AF.Silu, bias=cb[0:w_, pid:pid + 1]),
                 reads=[bkey(pb), "smallA"], writes=[f"QKm{par}_{pid}"])
            if which == 1:
                P.op("scalar", lambda e: e.activation(K32t[0:w_, pc, :], PS[0:w_, pb * 512:(pb + 1) * 512], AF.Silu, bias=cb[0:w_, pid:pid + 1]),
                     reads=[bkey(pb), "smallA"], writes=[f"K32{par}_{pc}"])

        def og_piece(q6):
            o_ = 1152 + q6 * 64
            for k in range(8):
                P.op("tensor", lambda e, k=k: e.matmul(PS[0:64, PROJ * 512:(PROJ + 1) * 512], lhsT=wm[:, k, o_:o_ + 64], rhs=xTa[:, k, tsl], start=(k == 0), stop=(k == 7)),
                     reads=[wmk, f"xTa{tt}"], writes=[bkey(PROJ)])
            P.op("scalar", lambda e: e.activation(ogt[0:64, q6, :], PS[0:64, PROJ * 512:(PROJ + 1) * 512], AF.Sigmoid, bias=bo[0:64, q6:q6 + 1]),
                 reads=[bkey(PROJ), "smallA"], writes=[f"og{par}_{q6}"])

        def v_if(cc):
            csl = slice(tt * TT + cc * 128, tt * TT + (cc + 1) * 128)
            for k in range(8):
                P.op("tensor", lambda e, k=k: e.matmul(PS[:, VIF * 512:VIF * 512 + 384], lhsT=xTa[:, k, csl], rhs=wm[:, k, 768:1152], start=(k == 0), stop=(k == 7)),
                     reads=[wmk, f"xTa{tt}"], writes=[bkey(VIF)])
            for k in range(8):
                P.op("tensor", lambda e, k=k: e.matmul(PS[:, VIF * 512 + 384:VIF * 512 + 388], lhsT=xTa[:, k, csl], rhs=wm[:, k, 1536:1540], start=(k == 0), stop=(k == 7)),
                     reads=[wmk, f"xTa{tt}"], writes=[bkey(VIF)])
            P.op("vector", lambda e: e.tensor_tensor(vat[:, cc, :, 0:192], PS[:, VIF * 512:VIF * 512 + 384].rearrange("p (h d) -> p h d", h=2), bvm[:].rearrange("p (h d) -> p h d", h=2), ALU.add),
                 reads=[bkey(VIF), "smallA", f"vaug{par}"], writes=[f"vaug{par}_{cc}"])
            P.op("vector", lambda e: e.tensor_tensor(ift[:, cc, :], PS[:, VIF * 512 + 384:VIF * 512 + 388], bif[:], ALU.add),
                 reads=[bkey(VIF), "smallA"], writes=[f"ifs{par}_{cc}"])

        def lpos_grp():
            P.op("scalar", lambda e: e.activation(lpt[:], ift[:, :, 2:4], AF.Exp, scale=-1.0), reads=[f"ifs{par}_{c_}" for c_ in range(4)], writes=[f"lpos{par}"])
            P.op("scalar", lambda e: e.activation(lpt[:], lpt[:], AF.Ln, bias=1.0), reads=[f"lpos{par}"], writes=[f"lpos{par}"])

        for cc in range(4):
            groups.append(lambda cc=cc: v_if(cc))
        groups.append(lpos_grp)
        for which in range(2):
            for pc in range(4):
                groups.append(lambda which=which, pc=pc: qk_piece(which, pc))
        for q6 in range(6):
            groups.append(lambda q6=q6: og_piece(q6))
        return groups

    GA, GBk = 2, 3

    def sub_step(tt, cc, sub):
        par = tt % 2
        c2 = cc % 2
        QKt, K32t, ogt, vat, ift, lpt = QKm[par], K32[par], og[par], vaug[par], ifs[par], lpos[par]
        csl = slice(cc * 128, (cc + 1) * 128)
        ga, gb = bkey(GA), bkey(GBk)
        brow2 = PS[:, GA * 512:GA * 512 + 256]
        st2 = PS[:, GA * 512 + 256:GA * 512 + 512]
        bL = PS[:, GA * 512 + 127:GA * 512 + 256:128]
        bcol = PS[:, GBk * 512 + 384:GBk * 512 + 386]
        v3 = lambda ap: ap.rearrange("p (h c) -> p h c", h=2)
        allq = [f"QKm{par}_{i}" for i in range(4)]
        allk = [f"QKm{par}_{i}" for i in range(4, 8)]
        if sub == 0:
            for h in range(2):
                P.op("vector", lambda e, h=h: e.tensor_scalar(LFb2[:, h, :], ones_f[:], lpt[:, cc, h:h + 1], None, ALU.mult), reads=[f"lpos{par}", "ones_f"], writes=["LFb2"])
            for h in range(2):
                P.op("tensor", lambda e, h=h: e.matmul(PS[:, GA * 512 + h * 128:GA * 512 + (h + 1) * 128], lhsT=LFb2[:, h, :], rhs=triU[:], start=True, stop=True), reads=["LFb2", "smallA"], writes=[ga])
            for h in range(2):
                kA, kB = QKt[:, 4 + 2 * h, csl], QKt[0:64, 4 + 2 * h + 1, csl]
                qA, qB = QKt[:, 2 * h, csl], QKt[0:64, 2 * h + 1, csl]
                st = PS[:, GA * 512 + 256 + h * 128:GA * 512 + 256 + (h + 1) * 128]
                P.op("tensor", lambda e, st=st, kA=kA, qA=qA: e.matmul(st, lhsT=kA, rhs=qA, start=True, stop=False), reads=allq + allk, writes=[ga])
                P.op("tensor", lambda e, st=st, kB=kB, qB=qB: e.matmul(st, lhsT=kB, rhs=qB, start=False, stop=True), reads=allq + allk, writes=[ga])
            P.op("tensor", lambda e: e.matmul(bcol, lhsT=triU[:], rhs=lpt[:, cc, :], start=True, stop=True), reads=[f"lpos{par}", "smallA"], writes=[gb])
            for h in range(2):
                ktp = PS[:, GBk * 512 + h * 192:GBk * 512 + (h + 1) * 192]
                P.op("tensor", lambda e, ktp=ktp, h=h: e.transpose(ktp[:, 0:128], K32t[:, 2 * h, csl], ident[:]), reads=[f"K32{par}_{2 * h}", "smallA"], writes=[gb])
                P.op("tensor", lambda e, ktp=ktp, h=h: e.transpose(ktp[:, 128:192], K32t[0:64, 2 * h + 1, csl], ident[0:64, 0:64]), reads=[f"K32{par}_{2 * h + 1}", "smallA"], writes=[gb])
        elif sub == 1:
            P.op("vector", lambda e: e.tensor_tensor(acolT[:, 0:2], ift[:, cc, 0:2], bcol, ALU.add), reads=[f"ifs{par}_{cc}", gb], writes=["acol0"])
            P.op("vector", lambda e: e.tensor_tensor(wpre2[:], acolT[:, 0:2], bL, ALU.subtract), reads=["acol0", ga], writes=["wpre2"])
            P.op("vector", lambda e: e.tensor_scalar(acolT[:, 2:4], acolT[:, 0:2], LNSC, None, ALU.add), reads=["acol0"], writes=["acol1"])
            P.op("vector", lambda e: e.scalar_tensor_tensor(DTa2[:], v3(brow2), -1.0, NEG2[:], ALU.mult, ALU.add), reads=[ga, "NEG2"], writes=["DTa2"])
        elif sub == 2:
            P.op("scalar", lambda e: e.activation(wcol2[c2][:], wpre2[:], AF.Exp), reads=["wpre2"], writes=[f"wcol2_{c2}"])
            P.op("scalar", lambda e: e.activation(dec2[c2][:], bL, AF.Exp, scale=-1.0), reads=[ga], writes=[f"dec2_{c2}"])
            P.op("scalar", lambda e: e.activation(Erow2[:], v3(brow2), AF.Exp, scale=-1.0, bias=lnsc[:]), reads=[ga, "lnsc"], writes=["Erow2"])
            for h in range(2):
                P.op("scalar", lambda e, h=h: e.activation(DT2[:, h, :], DTa2[:, h, :], AF.Exp, bias=acolT[:, 2 + h:3 + h]), reads=["DTa2", "acol1"], writes=["DT2"])
        elif sub == 3:
            P.op("vector", lambda e: e.tensor_tensor(scT2[c2][:], v3(st2), DT2[:], ALU.mult), reads=[ga, "DT2"], writes=[f"scT2_{c2}"])
            for h in range(2):
                ktp = PS[:, GBk * 512 + h * 192:GBk * 512 + (h + 1) * 192]
                P.op("vector", lambda e, h=h, ktp=ktp: e.tensor_scalar(kw2[c2][:, h, :], ktp, wcol2[c2][:, h:h + 1], None, ALU.mult), reads=[gb, f"wcol2_{c2}"], writes=[f"kw2_{c2}"])
            P.op("vector", lambda e: e.tensor_tensor(qsA2[c2][:], QKt[:, 0:4:2, csl], Erow2[:], ALU.mult), reads=allq + ["Erow2"], writes=[f"qsA2_{c2}"])
            P.op("gpsimd", lambda e: e.tensor_tensor(qsB2[c2][0:64], QKt[0:64, 1:4:2, csl], Erow2[0:64], ALU.mult), reads=allq + ["Erow2"], writes=[f"qsB2_{c2}"])
        elif sub == 4:
            for h in range(2):
                O, Dk = OB[h], DB[h]
                for o4 in range(4):
                    oc = slice(o4 * 64, (o4 + 1) * 64)
                    dst = PS[0:64, O * 512 + o4 * 128:O * 512 + (o4 + 1) * 128]
                    P.op("tensor", lambda e, dst=dst, oc=oc, h=h: e.matmul(dst, lhsT=vat[:, cc, h, oc], rhs=scT2[c2][:, h, :], start=True, stop=False),
                         reads=[f"vaug{par}_{cc}", f"vaug{par}", f"scT2_{c2}"], writes=[bkey(O)])
                    P.op("tensor", lambda e, dst=dst, oc=oc, h=h: e.matmul(dst, lhsT=Cb[h][0][:, oc], rhs=qsA2[c2][:, h, :], start=False, stop=False),
                         reads=[f"Cb_{h}0", f"qsA2_{c2}"], writes=[bkey(O)])
                    P.op("tensor", lambda e, dst=dst, oc=oc, h=h: e.matmul(dst, lhsT=Cb[h][1][0:64, oc], rhs=qsB2[c2][0:64, h, :], start=False, stop=True),
                         reads=[f"Cb_{h}1", f"qsB2_{c2}"], writes=[bkey(O)])
                dA = PS[:, Dk * 512:Dk * 512 + 256]
                dB = PS[0:64, Dk * 512 + 256:Dk * 512 + 512]
                P.op("tensor", lambda e, dA=dA, h=h: e.matmul(dA, lhsT=kw2[c2][:, h, 0:128], rhs=vat[:, cc, h, :], start=True, stop=True), reads=[f"kw2_{c2}", f"vaug{par}_{cc}", f"vaug{par}"], writes=[bkey(Dk)])
                P.op("tensor", lambda e, dB=dB, h=h: e.matmul(dB, lhsT=kw2[c2][:, h, 128:192], rhs=vat[:, cc, h, :], start=True, stop=True), reads=[f"kw2_{c2}", f"vaug{par}_{cc}", f"vaug{par}"], writes=[bkey(Dk)])
        elif sub == 5:
            for h in range(2):
                Dk = DB[h]
                dA = PS[:, Dk * 512:Dk * 512 + 256]
                dB = PS[0:64, Dk * 512 + 256:Dk * 512 + 512]
                P.op("vector", lambda e, dA=dA, h=h: e.scalar_tensor_tensor(C32[h][0][:], C32[h][0][:], dec2[c2][:, h:h + 1], dA, ALU.mult, ALU.add), reads=[bkey(Dk), f"dec2_{c2}"], writes=[f"C32_{h}0"])
                P.op("vector", lambda e, dB=dB, h=h: e.scalar_tensor_tensor(C32[h][1][0:64, :], C32[h][1][0:64, :], dec2[c2][0:64, h:h + 1], dB, ALU.mult, ALU.add), reads=[bkey(Dk), f"dec2_{c2}"], writes=[f"C32_{h}1"])
                P.op("scalar", lambda e, h=h: e.copy(Cb[h][0][:], C32[h][0][:]), reads=[f"C32_{h}0"], writes=[f"Cb_{h}0"])
                P.op("scalar", lambda e, h=h: e.copy(Cb[h][1][0:64, :], C32[h][1][0:64, :]), reads=[f"C32_{h}1"], writes=[f"Cb_{h}1"])
            den2 = PS[0:64, OB[0] * 512:(OB[0] + 2) * 512].rearrange("p (h c) -> p h c", h=2)[:, :, 384:512]
            P.op("scalar", lambda e: e.activation(rden2[0:64], den2, AF.Abs), reads=[bkey(OB[0]), bkey(OB[1])], writes=["rden2"])
            P.op("vector", lambda e: e.tensor_scalar(rden2[0:64], rden2[0:64], 1.0, None, ALU.max), reads=["rden2"], writes=["rden2"])
            P.op("vector", lambda e: e.reciprocal(rden2[0:64], rden2[0:64]), reads=["rden2"], writes=["rden2"])
        elif sub == 6:
            for p3 in range(3):
                num2 = PS[0:64, OB[0] * 512:(OB[0] + 2) * 512].rearrange("p (h c) -> p h c", h=2)[:, :, p3 * 128:(p3 + 1) * 128]
                P.op("gpsimd", lambda e, p3=p3: e.tensor_tensor(ogr2[0:64, p3], ogt[0:64, p3:6:3, csl], rden2[0:64], ALU.mult),
                     reads=[f"og{par}_{p3}", f"og{par}_{p3 + 3}", "rden2"], writes=[f"ogr2_{p3}"])
                P.op("vector", lambda e, p3=p3, num2=num2: e.tensor_tensor(yco2[0:64, :, p3, csl], num2, ogr2[0:64, p3], ALU.mult),
                     reads=[bkey(OB[0]), bkey(OB[1]), f"ogr2_{p3}"], writes=[f"yco_{p3}"])

    for g_ in proj_groups(0):
        g_()
    chunks = [(tt, cc) for tt in range(NTA) for cc in range(4)]
    nxt = []
    for sub in range(4):
        sub_step(0, 0, sub)
    for ci_, (tt, cc) in enumerate(chunks):
        if cc == 0:
            nxt = proj_groups(tt + 1) if tt + 1 < NTA else []
        nc_ = chunks[ci_ + 1] if ci_ + 1 < len(chunks) else None
        if nc_ is not None and nc_[1] == 0:
            for g_ in nxt:
                g_()
            nxt = []
        order = [(0, True), (4, False), (1, True), (5, False), (2, True), (6, False), (3, True)]
        for sub, is_next in order:
            if is_next:
                if nc_ is not None:
                    sub_step(nc_[0], nc_[1], sub)
            else:
                sub_step(tt, cc, sub)
            if nxt:
                nxt.pop(0)()
        if cc != 3:
            continue
        for h in range(2):
            for p3 in range(3):
                q = 3 * h + p3
                P.op("sync", lambda e, h=h, p3=p3, q=q, tt=tt: e.dma_start(out=T["ysend"][tt // 4].ap()[128 + 64 * q:128 + 64 * q + 64, (tt % 4) * TT:(tt % 4 + 1) * TT], in_=yco2[0:64, h, p3, :]),
                     reads=[f"yco_{p3}"], writes=["out_yc"], dma_key="outA")
    P.wait_res("sync", ["out_yc"])
    P.pop()
    P.pop()


BF = ml_dtypes.bfloat16
OFF = {}
_sizes = (768, 768, 768, 768, 768, 1536, 768, 768, 4, 4, 3072)
_names = ("a_q", "a_k", "a_v", "b_u", "b_v", "c_qk", "c_v", "c_o", "c_i", "c_f", "g")
_o = 0
for n_, s_ in zip(_names, _sizes):
    OFF[n_] = _o
    _o += s_


def const_tables():
    ident = np.eye(128, dtype=np.float32)
    triU = np.triu(np.ones((128, 128), np.float32))
    sel8 = np.zeros((8, 8 * 128), np.float32)
    for e in range(8):
        sel8[e, e * 128:(e + 1) * 128] = 1.0
    return dict(ident=ident, triU=triU, sel8=sel8)


def prep_B_weights(inp, layer):
    w_in, b_in = inp["w_in"][layer], inp["b_in"][layer]
    cols = np.r_[OFF["b_u"]:OFF["b_u"] + 768, OFF["b_v"]:OFF["b_v"] + 768, OFF["g"]:OFF["g"] + 3072]
    d = {}
    d["wB"] = np.ascontiguousarray(w_in[:, cols])
    d["bU"] = np.ascontiguousarray(b_in[OFF["b_u"]:OFF["b_u"] + 768].reshape(6, 128).T)
    d["bV"] = np.ascontiguousarray(b_in[OFF["b_v"]:OFF["b_v"] + 768].reshape(1, 768))
    d["bG"] = np.ascontiguousarray(b_in[OFF["g"]:OFF["g"] + 3072].reshape(24, 128).T)
    d["sgT"] = np.ascontiguousarray(inp["sg_w"][layer].transpose(0, 2, 1))
    d["sgb"] = np.ascontiguousarray(inp["sg_b"][layer].reshape(1, 768))
    d["slg"] = np.ascontiguousarray(inp["sg_ln_g"][layer].reshape(1, 768))
    d["slb"] = np.ascontiguousarray(inp["sg_ln_b"][layer].reshape(1, 768))
    wbr = np.concatenate([inp["w_br_a"][layer], inp["w_br_b"][layer], inp["w_br_c"][layer]], axis=0).reshape(14, 128, 1024)
    d["wbr"] = wbr
    d["wo"] = np.ascontiguousarray(inp["w_out"][layer])
    d["lng"] = np.ascontiguousarray(inp["ln_g"][layer].reshape(16, 128).T)
    d["lnb"] = np.ascontiguousarray(inp["ln_b"][layer].reshape(16, 128).T)
    j = layer // 2
    if layer % 2 == 0:
        d["w1"], d["w3"], d["w2"] = inp["ffn_w1"][j:j + 1], inp["ffn_w3"][j:j + 1], inp["ffn_w2"][j:j + 1]
    else:
        d["w1"], d["w3"], d["w2"] = inp["moe_w1"][j], inp["moe_w3"][j], inp["moe_w2"][j]
        d["rw"] = np.ascontiguousarray(inp["router_w"][j])
        d["rb"] = np.ascontiguousarray(inp["router_b"][j].reshape(1, 8))
    d.update(const_tables())
    return d


def yc_pieces_from_tokenmajor(y_c):
    n = y_c.shape[0]
    out = np.zeros((8, 128, n), y_c.dtype)
    for h in range(4):
        out[2 * h] = y_c[:, h * 192:h * 192 + 128].T
        out[2 * h + 1, 0:64] = y_c[:, h * 192 + 128:h * 192 + 192].T
    return out


def const_tables_A():
    p = np.arange(128)
    d = p % 64
    idx = (d % 32).astype(np.float32)
    invf = (np.float32(10000.0) ** (-idx * np.float32(2.0 / 64))).astype(np.float32).reshape(128, 1)
    sgn = np.where(d < 32, -1.0, 1.0).astype(np.float32).reshape(128, 1)
    perm = np.where(d < 32, p + 32, p - 32)
    permM = np.zeros((128, 128), np.float32)
    permM[perm, p] = 1.0
    triU = np.triu(np.ones((128, 128), np.float32))
    triL = np.tril(np.ones((128, 128), np.float32))
    maskA = np.concatenate([triU, triL, triU, triL], axis=1)
    return dict(ident=np.eye(128, dtype=np.float32), triU=triU, permM=permM, invf=invf, sgn=sgn, maskA=maskA)


def _pad128(v):
    out = np.zeros(128, np.float32)
    out[:v.shape[0]] = v
    return out


def prep_A_weights(inp, layer, hh):
    w_in, b_in = inp["w_in"][layer], inp["b_in"][layer]
    cols = []
    for g in range(3):
        h0 = 4 * g + 2 * hh
        for nm in ("a_q", "a_k", "a_v"):
            cols += list(range(OFF[nm] + h0 * 64, OFF[nm] + h0 * 64 + 128))
    m0 = 2 * hh
    cols += list(range(OFF["c_qk"] + m0 * 192, OFF["c_qk"] + m0 * 192 + 384))
    cols += list(range(OFF["c_qk"] + 768 + m0 * 192, OFF["c_qk"] + 768 + m0 * 192 + 384))
    cols += list(range(OFF["c_v"] + m0 * 192, OFF["c_v"] + m0 * 192 + 384))
    cols += list(range(OFF["c_o"] + m0 * 192, OFF["c_o"] + m0 * 192 + 384))
    cols += [OFF["c_i"] + m0, OFF["c_i"] + m0 + 1, OFF["c_f"] + m0, OFF["c_f"] + m0 + 1]
    cols = np.array(cols)
    assert cols.shape[0] == 2692
    d = {}
    d["wA"] = np.ascontiguousarray(w_in[:, cols])
    bA = b_in[cols]
    d["bqk"] = np.ascontiguousarray(np.stack([bA[g * 384 + w * 128:g * 384 + (w + 1) * 128] for g in range(3) for w in range(2)], axis=1))
    d["bva"] = np.ascontiguousarray(np.concatenate([bA[g * 384 + 256:g * 384 + 384] for g in range(3)]).reshape(1, 384))
    POFF, PW = [0, 128, 192, 320], [128, 64, 128, 64]
    d["bml"] = np.ascontiguousarray(np.stack([_pad128(bA[1152 + w * 384 + POFF[pc]:1152 + w * 384 + POFF[pc] + PW[pc]]) for w in range(2) for pc in range(4)], axis=1))
    d["bvm"] = np.ascontiguousarray(bA[1152 + 768:1152 + 1152].reshape(1, 384))
    d["bo"] = np.ascontiguousarray(np.stack([_pad128(bA[1152 + 1152 + q * 64:1152 + 1152 + (q + 1) * 64]) for q in range(6)], axis=1))
    d["bif"] = np.ascontiguousarray(bA[2688:2692].reshape(1, 4))
    cwl, cbl = inp["conv_w"][layer], inp["conv_b"][layer]
    cw = np.zeros((128, 8, 4), np.float32)
    cb = np.zeros((128, 8), np.float32)
    for w in range(2):
        for pc in range(4):
            ch0 = w * 768 + m0 * 192 + POFF[pc]
            cw[:PW[pc], w * 4 + pc, :] = cwl[:, ch0:ch0 + PW[pc]].T
            cb[:PW[pc], w * 4 + pc] = cbl[ch0:ch0 + PW[pc]]
    d["cw"], d["cb"] = cw, cb
    d.update(const_tables_A())
    return d


PAIRS = [[0, 1], [2, 3], [4, 5], [6, 7]]
A_LAYER = dict(wA=([1024, 2692], F32), bqk=([128, 6], F32), bva=([1, 384], F32), bml=([128, 8], F32), bvm=([1, 384], F32),
               bif=([1, 4], F32), cw=([128, 8, 4], F32), cb=([128, 8], F32), bo=([128, 6], F32))
B_LAYER = dict(wB=([1024, 4608], F32), bU=([128, 6], F32), bV=([1, 768], F32), bG=([128, 24], F32),
               sgT=([6, 128, 128], F32), sgb=([1, 768], F32), slg=([1, 768], F32), slb=([1, 768], F32),
               wbr=([14, 128, 1024], F32), wo=([1024, 1024], F32), lng=([128, 16], F32), lnb=([128, 16], F32))
CONSTS = dict(ident=([128, 128], F32), triU=([128, 128], F32), permM=([128, 128], F32), invf=([128, 1], F32), sgn=([128, 1], F32),
              maskA=([128, 512], F32), sel8=([8, 1024], F32))


def _input_shapes():
    sh = dict(x0T=([1024, 4096], F32), x0h=([1024, 2048], F32), pos=([1, 4096], I32), selh=([128, 2], F32))
    sh.update(CONSTS)
    for l in range(2):
        for k, v in A_LAYER.items():
            sh[f"{k}_A{l}"] = v
        for k, v in B_LAYER.items():
            sh[f"{k}_B{l}"] = v
        E = 8 if l == 1 else 1
        sh[f"w1_B{l}"] = ([E, 1024, 2816], F32)
        sh[f"w3_B{l}"] = ([E, 1024, 2816], F32)
        sh[f"w2_B{l}"] = ([E, 2816, 1024], F32)
    sh["rw_B1"] = ([1024, 8], F32)
    sh["rb_B1"] = ([1, 8], F32)
    return sh


def _build_fused():
    nc = bass.Bass("TRN2", target_bir_lowering=False)
    shapes = _input_shapes()
    I = {k: nc.dram_tensor(k, s, dt, kind="ExternalInput") for k, (s, dt) in shapes.items()}
    out = nc.dram_tensor("out", [1024, 2048], F32, kind="ExternalOutput")
    ysend = [nc.dram_tensor(f"ysend{i}", [512, 2048], BF16) for i in range(2)]
    ygath = [nc.dram_tensor(f"ygath{i}", [1024, 2048], BF16) for i in range(2)]
    x1s = nc.dram_tensor("x1_scr", [1024, 2048], F32)
    xbs = [nc.dram_tensor(f"xbs{i}", [512, 2048], BF16) for i in range(2)]
    xg = [nc.dram_tensor(f"xg{i}", [1024, 2048], BF16) for i in range(2)]

    def allgather(P, src, dst, reads, writes, key):
        P.op("gpsimd", lambda e: e.collective_compute("AllGather", ALU.bypass, replica_groups=PAIRS, ins=[src.ap().opt()], outs=[dst.ap().opt()]),
             reads=reads, writes=writes, dma_key=key, sem_inc=1)

    with ExitStack() as st:
        P = Prog(nc, st)

        def load_wuv(l, buf):
            wB_ = I[f"wB_B{l}"].ap()
            for c in range(8):
                P.op("gpsimd", lambda e, c=c: e.dma_start(out=buf[:, c, :], in_=wB_[c * 128:(c + 1) * 128, 0:1536]),
                     writes=[f"wUV{c}"], dma_key=f"wUVld{c}")

        for l in range(2):
            TA = {k: I[k] for k in CONSTS}
            TA.update({k: I[f"{k}_A{l}"] for k in A_LAYER})
            TA["pos"] = I["pos"]
            TA["xsrc"] = ("ext", I["x0T"]) if l == 0 else ("gath", xg[0], xg[1])
            TA["ysend"] = ysend
            build_A(nc, P, TA, tag=f"A{l}")
            TB = {k: I[k] for k in CONSTS}
            TB.update({k: I[f"{k}_B{l}"] for k in B_LAYER})
            for k in ("w1", "w3", "w2"):
                TB[k] = I[f"{k}_B{l}"]
            if l == 1:
                TB["rw"], TB["rb"] = I["rw_B1"], I["rb_B1"]
            TB["selh"] = I["selh"]
            TB["ygath"] = ygath
            TB["x_in"] = I["x0h"] if l == 0 else x1s
            TB["xo"] = x1s if l == 0 else out
            TB["xob"] = xbs if l == 0 else None
            P.push()
            P.prefix = f"BL{l}_"
            wuv_l = P.sb([128, 8, 1536], BF16, name="wuv")
            for i in range(2):
                allgather(P, ysend[i], ygath[i], ["out_ya", "out_yc"], [f"ygath{i}"], f"ccy{i}")
            load_wuv(l, wuv_l)
            TB["wuv_buf"] = wuv_l
            for half in range(2):
                build_B(nc, P, TB, l == 1, half, tag=f"B{l}")
            P.pop()
            if l == 0:
                for i in range(2):
                    allgather(P, xbs[i], xg[i], ["out_xb"], ["xg"], f"ccx{i}")
        P.wait_res("sync", ["out_x"])
        P.emit()
    return nc, list(shapes.keys())


def kernel(**inputs):
    inp = {k: np.asarray(v) for k, v in inputs.items()}
    cores = list(range(8))
    nc, names = _build_fused()
    consts = const_tables_A()
    consts["sel8"] = const_tables()["sel8"]
    wA = [[prep_A_weights(inp, l, hh) for hh in range(2)] for l in range(2)]
    wBs = [prep_B_weights(inp, l) for l in range(2)]
    maps = []
    for c in cores:
        b, r = c // 2, c % 2
        xT = np.ascontiguousarray(inp["x"][b].T)
        m = dict(x0T=xT, x0h=np.ascontiguousarray(xT[:, r * 2048:(r + 1) * 2048]),
                 pos=np.ascontiguousarray(inp["positions"][b:b + 1]).astype(np.int32))
        sel = np.zeros((128, 2), np.float32)
        sel[:, r] = 1.0
        m["selh"] = sel
        for k in CONSTS:
            m[k] = consts[k]
        for l in range(2):
            for k in A_LAYER:
                m[f"{k}_A{l}"] = wA[l][r][k]
            for k in list(B_LAYER) + ["w1", "w3", "w2"]:
                m[f"{k}_B{l}"] = wBs[l][k]
        m["rw_B1"], m["rb_B1"] = wBs[1]["rw"], wBs[1]["rb"]
        maps.append({k: np.ascontiguousarray(m[k]) for k in names})
    res = run_bass_kernel_spmd(nc, maps, core_ids=cores).results
    out = np.empty((4, 4096, 1024), np.float32)
    for c in cores:
        b, r = c // 2, c % 2
        out[b, r * 2048:(r + 1) * 2048, :] = res[c]["out"].T
    return out
```

```python
import math, os
import numpy as np
import ml_dtypes
from contextlib import ExitStack
import concourse.bass as bass
import concourse.mybir as mybir
from concourse.bass_utils import run_bass_kernel_spmd


F32 = mybir.dt.float32
BF16 = mybir.dt.bfloat16
I32 = mybir.dt.int32
AF = mybir.ActivationFunctionType
ALU = mybir.AluOpType
AX = mybir.AxisListType

ENGS = ["sync", "scalar", "vector", "gpsimd", "tensor"]
SAME_ENG_SYNC = {"scalar": True, "vector": True, "gpsimd": True, "tensor": False, "sync": False}


class Prog:
    def __init__(self, nc, stack):
        self.nc = nc
        self.stack = stack
        self.ops = {e: [] for e in ENGS}
        self.res = {}
        self.dma_count = {}
        self.seen = {e: {} for e in ENGS}
        self.nsb = 0
        self.stacks = [stack]
        self.last_real = {}

    def sb(self, shape, dtype, name=None):
        self.nsb += 1
        name = "s_" + getattr(self, "prefix", "") + (name or f"sb{self.nsb}")
        return self.stacks[-1].enter_context(self.nc.sbuf_tensor(name, list(shape), dtype))

    def ps(self, shape, dtype, name=None):
        self.nsb += 1
        name = "p_" + getattr(self, "prefix", "") + (name or f"ps{self.nsb}")
        return self.stacks[-1].enter_context(self.nc.psum_tensor(name, list(shape), dtype))

    def _need(self, eng, tok, waits):
        if tok is None:
            return
        if tok[0] == "e":
            _, e2, idx = tok
            if e2 == eng and not SAME_ENG_SYNC[eng]:
                return
            k = ("e", e2)
            if self.seen[eng].get(k, -1) >= idx:
                return
            if waits.get(k, -1) < idx:
                waits[k] = idx
        else:
            _, key, cnt = tok
            cnt = self.dma_count[key]
            k = ("d", key)
            if self.seen[eng].get(k, -1) >= cnt:
                return
            if waits.get(k, -1) < cnt:
                waits[k] = cnt

    def op(self, eng, fn, reads=(), writes=(), dma_key=None, sem_inc=16):
        excl = [r for r in reads if r.startswith("ps") and r not in writes]
        if excl:
            writes = list(writes) + excl
        waits = {}
        for r in reads:
            st = self.res.get(r)
            if st is not None:
                self._need(eng, st["w"], waits)
        for w in writes:
            st = self.res.get(w)
            if st is not None:
                self._need(eng, st["w"], waits)
                for t in st["r"].values():
                    self._need(eng, t, waits)
        for k, v in waits.items():
            self.seen[eng][k] = v
        idx = len(self.ops[eng])
        if dma_key is not None:
            self.dma_count[dma_key] = self.dma_count.get(dma_key, 0) + 1
            self.key_inc = getattr(self, "key_inc", {})
            self.key_inc[dma_key] = sem_inc
            tok = ("d", dma_key, self.dma_count[dma_key])
            rk = ("d", dma_key)
        else:
            tok = ("e", eng, idx)
            rk = ("e", eng)
        self.ops[eng].append({"fn": fn, "waits": waits, "dma_key": dma_key, "sig": False})
        if fn is not None and dma_key is None:
            self.last_real[eng] = idx
        if fn is not None:
            for r in reads:
                st = self.res.setdefault(r, {"w": None, "r": {}})
                st["r"][rk] = tok
        for w in writes:
            self.res[w] = {"w": tok, "r": {}}
        return tok

    def barrier(self, dma=True):
        toks = [("e", e2, i) for e2, i in self.last_real.items()]
        if dma:
            toks += [("d", k, c) for k, c in self.dma_count.items()]
        for e in ENGS:
            waits = {}
            for t in toks:
                self._need(e, t, waits)
            if waits:
                for k, v in waits.items():
                    self.seen[e][k] = v
                self.ops[e].append({"fn": None, "waits": waits, "dma_key": None, "sig": False})

    def scope(self):
        P = self

        class _S:
            def __enter__(s_):
                s_.st = ExitStack()
                s_.st.__enter__()
                P.stacks.append(s_.st)

            def __exit__(s_, *a):
                P.barrier()
                P.stacks.pop()
                return s_.st.__exit__(*a)
        return _S()

    def push(self):
        st = ExitStack()
        st.__enter__()
        self.stacks.append(st)

    def pop(self, dma=True):
        self.barrier(dma)
        st = self.stacks.pop()
        st.__exit__(None, None, None)

    def wait_res(self, eng, reads):
        self.op(eng, None, reads=reads)

    def emit(self):
        nc = self.nc
        for e in ENGS:
            for o in self.ops[e]:
                for (kind, k), v in o["waits"].items():
                    if kind == "e":
                        self.ops[k][v]["sig"] = True
        sigcnt = {}
        for e in ENGS:
            c = 0
            arr = []
            for o in self.ops[e]:
                if o["sig"]:
                    c += 1
                arr.append(c)
            sigcnt[e] = arr
        esem = {e: self.stack.enter_context(nc.semaphore("es_" + e)) for e in ENGS}
        dsem = {k: self.stack.enter_context(nc.semaphore("ds_%d" % i)) for i, k in enumerate(self.dma_count)}
        self.n_sems = len(esem) + len(dsem)
        block = self.stack.enter_context(nc.Block())

        def make(e):
            def body(eng):
                for o in self.ops[e]:
                    for (kind, k), v in o["waits"].items():
                        if kind == "e":
                            eng.wait_ge(esem[k], sigcnt[k][v])
                        else:
                            eng.wait_ge(dsem[k], self.key_inc[k] * v)
                    if o["fn"] is None:
                        continue
                    inst = o["fn"](eng)
                    if o["dma_key"] is not None:
                        inst.then_inc(dsem[o["dma_key"]], self.key_inc[o["dma_key"]])
                    elif o["sig"]:
                        inst.then_inc(esem[e], 1)
            return body

        for e in ENGS:
            if self.ops[e]:
                getattr(block, e)(make(e))


D = 1024
NT = 1024
TT = 512
NTT = NT // TT
DFF = 2816
NJ = DFF // 128
ALPHA = 4.0 ** 0.25
EPS = 1e-5


class WStream:
    def __init__(self, P, name, shape, nbuf=2, dtype=BF16, eng="gpsimd"):
        self.P, self.name, self.nbuf, self.eng = P, name, nbuf, eng
        self.bufs = [P.sb(shape, dtype, name=f"{name}_{i}") for i in range(nbuf)]
        self.i = 0

    def next(self):
        k = self.i % self.nbuf
        self.i += 1
        return k, self.bufs[k], f"{self.name}{k}"

    def load(self, k, dst_ap, src_ap, eng=None):
        key = f"{self.name}{k}"
        self.P.op(eng or self.eng, lambda e: e.dma_start(out=dst_ap, in_=src_ap), writes=[key], dma_key=key)


def build_B(nc, P, T, moe, half, tag="B"):
    E = 8 if moe else 1
    hs = slice(half * NT, (half + 1) * NT)
    P.push()
    P.prefix = f"{tag}h{half}_"
    PS = P.ps([128, 4096], F32, name="psB")
    bank = lambda i: PS[:, i * 512:(i + 1) * 512]
    bkey = lambda i: f"psB{i}"

    X32 = P.sb([128, 8, NT], F32, name="X32")
    Xbf = P.sb([128, 8, NT], BF16, name="Xbf")
    xk = lambda c, t: f"x32_{c}_{t}"
    xbk = lambda c, t: f"xbf_{c}_{t}"
    allx = [xk(c, t) for c in range(8) for t in range(NTT)]
    allxb = [xbk(c, t) for c in range(8) for t in range(NTT)]
    xT = T["x_in"].ap()[:, hs].rearrange("(c p) n -> p c n", p=128)
    wB = T["wB"].ap()
    for c in range(8):
        P.op("sync", lambda e, c=c: e.dma_start(out=X32[:, c, :], in_=xT[:, c, :]),
             reads=["out_x"], writes=[xk(c, t) for t in range(NTT)], dma_key=f"x32ld{c}")
        P.op("scalar", lambda e, c=c: e.copy(Xbf[:, c, :], X32[:, c, :]),
             reads=[xk(c, t) for t in range(NTT)], writes=[xbk(c, t) for t in range(NTT)])
    def small(name, shape, src, dtype=F32, eng="sync", grp="small"):
        t = P.sb(shape, dtype, name=name)
        if eng == "gpsimd":
            grp = grp + "_g"
        P.op(eng, lambda e: e.dma_start(out=t[:], in_=src), writes=[grp], dma_key=grp)
        return t

    ident = small("ident", [128, 128], T["ident"].ap())
    triU = small("triU", [128, 128], T["triU"].ap())
    bU = small("bU", [128, 6], T["bU"].ap())
    bG = small("bG", [128, 24], T["bG"].ap())
    lng = small("lng", [128, 16], T["lng"].ap())
    lnb = small("lnb", [128, 16], T["lnb"].ap())
    onesM = P.sb([128, 128], F32, name="onesM")
    P.op("vector", lambda e: e.memset(onesM[:], 1.0 / D), writes=["onesM"])
    epsc = P.sb([128, 1], F32, name="epsc")
    P.op("vector", lambda e: e.memset(epsc[:], EPS), writes=["epsc"])
    yb = P.sb([128, 6, NT], BF16, name="yb")
    P.push()
    Y8 = P.sb([128, 8, NT], BF16, name="Y8")
    P.push()
    wuv = T["wuv_buf"]
    bVb = small("bVb", [128, 768], T["bV"].ap().partition_broadcast(128))
    slg = small("slg", [128, 768], T["slg"].ap().partition_broadcast(128))
    slb = small("slb", [128, 768], T["slb"].ap().partition_broadcast(128))
    sgb = small("sgb", [1, 768], T["sgb"].ap(), dtype=BF16, eng="gpsimd")
    bV1 = small("bV1", [1, 768], T["bV"].ap(), dtype=BF16, eng="gpsimd")
    sgT32 = small("sgT32", [128, 6, 128], T["sgT"].ap().rearrange("g s t -> s g t"))
    ones_r = P.sb([1, 128], BF16, name="ones_r")
    P.op("vector", lambda e: e.memset(ones_r[:], 1.0), writes=["ones_r"])
    sgTb = P.sb([128, 6, 128], BF16, name="sgTb")
    for g in range(6):
        P.op("vector", lambda e, g=g: e.tensor_tensor(sgTb[:, g, :], sgT32[:, g, :], triU[:], ALU.mult),
             reads=["small"], writes=[f"sgTb{g}"])

    cand = [P.sb([128, 8, NT], BF16, name=f"ycand{i}") for i in range(2)]
    selh = small("selh", [128, 2], T["selh"].ap(), grp="small3")
    for i in range(2):
        yg = T["ygath"][i].ap()
        for q in range(8):
            r0 = q * 512 if q < 2 else ((q - 2) // 3) * 512 + 128 + ((q - 2) % 3) * 128
            P.op("sync", lambda e, i=i, q=q, r0=r0, yg=yg: e.dma_start(out=cand[i][:, q, :], in_=yg[r0:r0 + 128, hs]),
                 reads=[f"ygath{i}"], writes=[f"ycand{i}"], dma_key="yld")
    gv = [P.sb([128, 768], F32, name=f"gv{i}") for i in range(2)]
    vn = P.sb([128, 4, 768], BF16, name="vn")
    usb = P.sb([128, 6, TT], BF16, name="usb")
    stats = P.sb([128, 2, 6], F32, name="bnst")
    mv = P.sb([128, 2], F32, name="bnmv")
    rstd = P.sb([128, 1], F32, name="rstdv")
    ui = 0
    for tt in range(NTT):
        tsl = slice(tt * TT, (tt + 1) * TT)
        for g in range(6):
            b = ui % 2
            ui += 1
            for k in range(8):
                P.op("tensor", lambda e, b=b, k=k, g=g, tsl=tsl: e.matmul(bank(b), lhsT=wuv[:, k, g * 128:(g + 1) * 128], rhs=Xbf[:, k, tsl], start=(k == 0), stop=(k == 7)),
                     reads=[f"wUV{k}", xbk(k, tt)], writes=[bkey(b)])
            P.op("scalar", lambda e, b=b, g=g: e.activation(usb[:, g, :], bank(b), AF.Gelu_apprx_tanh, bias=bU[:, g:g + 1]),
                 reads=[bkey(b), "small"], writes=[f"usb{g}"])
        for cc in range(4):
            ch = tt * 4 + cc
            csl = slice(ch * 128, (ch + 1) * 128)
            b0 = 2 + 2 * (cc % 2)
            for k in range(8):
                P.op("tensor", lambda e, b0=b0, k=k, csl=csl: e.matmul(bank(b0), lhsT=Xbf[:, k, csl], rhs=wuv[:, k, 768:1280], start=(k == 0), stop=False),
                     reads=[f"wUV{k}", xbk(k, tt)], writes=[bkey(b0)])
            P.op("tensor", lambda e, b0=b0: e.matmul(bank(b0), lhsT=ones_r[0:1, :], rhs=bV1[0:1, 0:512], start=False, stop=True),
                 reads=["ones_r", "small_g"], writes=[bkey(b0)])
            for k in range(8):
                P.op("tensor", lambda e, b0=b0, k=k, csl=csl: e.matmul(PS[:, (b0 + 1) * 512:(b0 + 1) * 512 + 256], lhsT=Xbf[:, k, csl], rhs=wuv[:, k, 1280:1536], start=(k == 0), stop=False),
                     reads=[f"wUV{k}", xbk(k, tt)], writes=[bkey(b0 + 1)])
            P.op("tensor", lambda e, b0=b0: e.matmul(PS[:, (b0 + 1) * 512:(b0 + 1) * 512 + 256], lhsT=ones_r[0:1, :], rhs=bV1[0:1, 512:768], start=False, stop=True),
                 reads=["ones_r", "small_g"], writes=[bkey(b0 + 1)])
            g_ = gv[cc % 2]
            gk = f"gv{cc % 2}"
            P.op("scalar", lambda e, b0=b0, g_=g_: e.activation(g_[:, 0:512], bank(b0), AF.Gelu_apprx_tanh),
                 reads=[bkey(b0)], writes=[gk])
            P.op("scalar", lambda e, b0=b0, g_=g_: e.activation(g_[:, 512:768], PS[:, (b0 + 1) * 512:(b0 + 1) * 512 + 256], AF.Gelu_apprx_tanh),
                 reads=[bkey(b0 + 1)], writes=[gk])
            P.op("vector", lambda e, g_=g_: e.bn_stats(stats[:, 0, :], g_[:, 0:384]), reads=[gk], writes=["bnst0"])
            P.op("vector", lambda e, g_=g_: e.bn_stats(stats[:, 1, :], g_[:, 384:768]), reads=[gk], writes=["bnst1"])
            P.op("vector", lambda e: e.bn_aggr(mv[:], stats[:]), reads=["bnst0", "bnst1"], writes=["bnmv"])
            P.op("scalar", lambda e: e.activation(rstd[:], mv[:, 1:2], AF.Sqrt, bias=epsc[:]), reads=["bnmv", "epsc"], writes=["rstdv"])
            P.op("vector", lambda e: e.reciprocal(rstd[:], rstd[:]), reads=["rstdv"], writes=["rstdv"])
            P.op("vector", lambda e, g_=g_: e.tensor_scalar(g_[:], g_[:], mv[:, 0:1], rstd[:], ALU.subtract, ALU.mult),
                 reads=[gk, "bnmv", "rstdv"], writes=[gk])
            P.op("gpsimd", lambda e, g_=g_: e.tensor_tensor(g_[:], g_[:], slg[:], ALU.mult), reads=[gk, "small"], writes=[gk])
            P.op("gpsimd", lambda e, g_=g_, cc=cc: e.tensor_tensor(vn[:, cc, :], g_[:], slb[:], ALU.add), reads=[gk, "small"], writes=[f"vn{cc}"])
        for g in range(6):
            b = 6 + g % 2
            for cc in range(4):
                osl = slice(cc * 128, (cc + 1) * 128)
                P.op("tensor", lambda e, b=b, g=g, cc=cc, osl=osl: e.matmul(PS[:, b * 512 + cc * 128:b * 512 + (cc + 1) * 128], lhsT=vn[:, cc, g * 128:(g + 1) * 128], rhs=sgTb[:, g, :], start=True, stop=False),
                     reads=[f"vn{cc}", f"sgTb{g}"], writes=[bkey(b)])
                P.op("tensor", lambda e, b=b, g=g, cc=cc, osl=osl: e.matmul(PS[:, b * 512 + cc * 128:b * 512 + (cc + 1) * 128], lhsT=ones_r[0:1, :], rhs=sgb[0:1, g * 128:(g + 1) * 128], start=False, stop=True),
                     reads=["ones_r", "small_g"], writes=[bkey(b)])
            P.op("vector", lambda e, b=b, g=g, tsl=tsl: e.tensor_tensor(yb[:, g, tsl], bank(b), usb[:, g, :], ALU.mult),
                 reads=[bkey(b), f"usb{g}"], writes=[f"yb{g}_{tt}"])

    if T.get("after_S1_hook") is not None and half == 1:
        T["after_S1_hook"]()
    P.op("vector", lambda e: e.tensor_scalar(Y8[:], cand[0][:], selh[:, 0:1], None, ALU.mult), reads=["ycand0", "small3"], writes=["Y8"])
    P.op("vector", lambda e: e.scalar_tensor_tensor(Y8[:], cand[1][:], selh[:, 1:2], Y8[:], ALU.mult, ALU.add), reads=["ycand1", "small3", "Y8"], writes=["Y8"])
    P.pop()
    P.push()
    Z = P.sb([128, 8, NT], BF16, name="Z")
    wG = WStream(P, "wG", [128, 3, 8, 128], nbuf=2)
    wBR = WStream(P, "wBR", [128, 14, 128], nbuf=2)
    wbr = T["wbr"].ap()

    def load_z(m):
        kg, g_t, gkey = wG.next()
        for br in range(3):
            c0 = 1536 + br * 1024 + m * 128
            wG.load(kg, g_t[:, br, :, :], wB[:, c0:c0 + 128].rearrange("(k p) n -> p k n", p=128))
        kb, b_t, bkey_ = wBR.next()
        wBR.load(kb, b_t[:], wbr[:, :, m * 128:(m + 1) * 128].rearrange("q p n -> p q n"))
        return g_t, gkey, b_t, bkey_

    sig = [P.sb([128, TT], F32, name=f"sig{i}") for i in range(2)]
    zt = [P.sb([128, TT], F32, name=f"zt{i}") for i in range(2)]
    zacc = [P.sb([128, TT], F32, name=f"zacc{i}") for i in range(2)]
    nxt = load_z(0)
    pi = 0
    zi = 0
    brK = [[(0, 128, "Y8", lambda tsl, q=q: Y8[:, q, tsl]) for q in range(2)],
           [(0, 128, f"yb{g}", lambda tsl, g=g: yb[:, g, tsl]) for g in range(6)],
           [(0, 128, "Y8", lambda tsl, pc=pc: Y8[:, 2 + pc, tsl]) for pc in range(6)]]
    brOff = [0, 2, 8]
    wO = WStream(P, "wO", [128, 8, 1024], nbuf=1)
    ko, wo, wok = wO.next()
    for m in range(8):
        g_t, gkey, b_t, bkey_ = nxt
        if m + 1 < 8:
            nxt = load_z(m + 1)
        if m == 2:
            for k in range(8):
                wO.load(ko, wo[:, k, :], T["wo"].ap()[k * 128:(k + 1) * 128, :])
        for tt in range(NTT):
            tsl = slice(tt * TT, (tt + 1) * TT)
            za = zacc[zi % 2]
            zk = f"zacc{zi % 2}"
            zi += 1
            for br in range(3):
                bG_ = (2 * pi) % 8
                bP_ = bG_ + 1
                pi += 1
                for k in range(8):
                    P.op("tensor", lambda e, bG_=bG_, br=br, k=k, tsl=tsl, g_t=g_t: e.matmul(bank(bG_), lhsT=g_t[:, br, k, :], rhs=Xbf[:, k, tsl], start=(k == 0), stop=(k == 7)),
                         reads=[gkey, xbk(k, tt)], writes=[bkey(bG_)])
                pcs = brK[br]
                for qi, (p0, rows, rkey, apf) in enumerate(pcs):
                    rk = rkey if br != 1 else f"{rkey}_{tt}"
                    P.op("tensor", lambda e, bP_=bP_, qi=qi, rows=rows, apf=apf, tsl=tsl, br=br, b_t=b_t, n=len(pcs): e.matmul(bank(bP_), lhsT=b_t[0:rows, brOff[br] + qi, :], rhs=apf(tsl), start=(qi == 0), stop=(qi == n - 1)),
                         reads=[bkey_, rk], writes=[bkey(bP_)])
                s_ = sig[pi % 2]
                sk = f"sig{pi % 2}"
                P.op("scalar", lambda e, bG_=bG_, s_=s_, br=br, m=m: e.activation(s_[:], bank(bG_), AF.Sigmoid, bias=bG[:, br * 8 + m:br * 8 + m + 1]),
                     reads=[bkey(bG_), "small"], writes=[sk])
                if br == 0:
                    P.op("vector", lambda e, s_=s_, bP_=bP_, za=za: e.tensor_tensor(za[:], s_[:], bank(bP_), ALU.mult),
                         reads=[sk, bkey(bP_)], writes=[zk])
                else:
                    z_ = zt[pi % 2]
                    ztk = f"zt{pi % 2}"
                    P.op("vector", lambda e, s_=s_, bP_=bP_, z_=z_: e.tensor_tensor(z_[:], s_[:], bank(bP_), ALU.mult),
                         reads=[sk, bkey(bP_)], writes=[ztk])
                    if br == 1:
                        P.op("vector", lambda e, z_=z_, za=za: e.tensor_tensor(za[:], za[:], z_[:], ALU.add),
                             reads=[ztk, zk], writes=[zk])
                    else:
                        P.op("vector", lambda e, z_=z_, za=za, m=m, tsl=tsl: e.tensor_tensor(Z[:, m, tsl], za[:], z_[:], ALU.add),
                             reads=[ztk, zk], writes=[f"Z{m}_{tt}"])

    bi = 0
    for tt in range(NTT):
        tsl = slice(tt * TT, (tt + 1) * TT)
        for m in range(8):
            b = bi % 8
            bi += 1
            for k in range(8):
                P.op("tensor", lambda e, b=b, k=k, m=m, tsl=tsl: e.matmul(bank(b), lhsT=wo[:, k, m * 128:(m + 1) * 128], rhs=Z[:, k, tsl], start=(k == 0), stop=(k == 7)),
                     reads=[wok, f"Z{k}_{tt}"], writes=[bkey(b)])
            P.op("vector", lambda e, b=b, m=m, tsl=tsl: e.scalar_tensor_tensor(X32[:, m, tsl], X32[:, m, tsl], ALPHA, bank(b), ALU.mult, ALU.add),
                 reads=[bkey(b), xk(m, tt)], writes=[xk(m, tt)])

    P.pop()
    P.pop()
    sq = [P.sb([128, TT], F32, name=f"sq{i}") for i in range(2)]
    mean_sb = [P.sb([128, TT], F32, name=f"mean_sb{i}") for i in range(NTT)]
    rstd_sb = [P.sb([128, TT], F32, name=f"rstd_sb{i}") for i in range(NTT)]
    lt = [P.sb([128, TT], F32, name=f"lt{i}") for i in range(2)]

    def layer_norm(which, write_bf=True, bbase=0):
        for tt in range(NTT):
            tsl = slice(tt * TT, (tt + 1) * TT)
            b1, b2 = bbase + 2 * (tt % 2), bbase + 2 * (tt % 2) + 1
            for c in range(8):
                P.op("tensor", lambda e, c=c, tsl=tsl, b1=b1: e.matmul(bank(b1), lhsT=onesM[:], rhs=X32[:, c, tsl], start=(c == 0), stop=(c == 7)),
                     reads=["onesM", xk(c, tt)], writes=[bkey(b1)])
            for c in range(8):
                s_ = sq[c % 2]
                P.op("scalar", lambda e, c=c, tsl=tsl, s_=s_: e.activation(s_[:], X32[:, c, tsl], AF.Square),
                     reads=[xk(c, tt)], writes=[f"sq{c % 2}"])
                P.op("tensor", lambda e, c=c, s_=s_, b2=b2: e.matmul(bank(b2), lhsT=onesM[:], rhs=s_[:], start=(c == 0), stop=(c == 7)),
                     reads=["onesM", f"sq{c % 2}"], writes=[bkey(b2)])
        for tt in range(NTT):
            b1, b2 = bbase + 2 * (tt % 2), bbase + 2 * (tt % 2) + 1
            m_, r_ = mean_sb[tt], rstd_sb[tt]
            mk_, rk_ = f"mean_sb{tt}", f"rstd_sb{tt}"
            P.op("scalar", lambda e, b1=b1, m_=m_: e.copy(m_[:], bank(b1)), reads=[bkey(b1)], writes=[mk_])
            P.op("vector", lambda e, m_=m_, r_=r_: e.tensor_tensor(r_[:], m_[:], m_[:], ALU.mult), reads=[mk_], writes=[rk_])
            P.op("vector", lambda e, b2=b2, r_=r_: e.tensor_tensor(r_[:], bank(b2), r_[:], ALU.subtract), reads=[bkey(b2), rk_], writes=[rk_])
            P.op("scalar", lambda e, r_=r_: e.activation(r_[:], r_[:], AF.Sqrt, bias=epsc[:]), reads=[rk_, "epsc"], writes=[rk_])
            P.op("vector", lambda e, r_=r_: e.reciprocal(r_[:], r_[:]), reads=[rk_], writes=[rk_])
        li = 0
        for tt in range(NTT):
            tsl = slice(tt * TT, (tt + 1) * TT)
            m_, r_ = mean_sb[tt], rstd_sb[tt]
            mk_, rk_ = f"mean_sb{tt}", f"rstd_sb{tt}"
            for c in range(8):
                l_ = lt[li % 2]
                lk = f"lt{li % 2}"
                li += 1
                P.op("vector", lambda e, c=c, tsl=tsl, l_=l_, m_=m_: e.tensor_tensor(l_[:], X32[:, c, tsl], m_[:], ALU.subtract),
                     reads=[xk(c, tt), mk_], writes=[lk])
                P.op("gpsimd", lambda e, l_=l_, r_=r_: e.tensor_tensor(l_[:], l_[:], r_[:], ALU.mult),
                     reads=[lk, rk_], writes=[lk])
                col = which * 8 + c
                P.op("vector", lambda e, c=c, tsl=tsl, l_=l_, col=col: e.tensor_scalar(X32[:, c, tsl], l_[:], lng[:, col:col + 1], lnb[:, col:col + 1], ALU.mult, ALU.add),
                     reads=[lk, "small"], writes=[xk(c, tt)])
                if write_bf:
                    P.op("scalar", lambda e, c=c, tsl=tsl: e.copy(Xbf[:, c, tsl], X32[:, c, tsl]),
                         reads=[xk(c, tt)], writes=[xbk(c, tt)])

    w13 = WStream(P, "w13", [128, 2, 8, 256], nbuf=2)
    w1d, w3d, w2d = T["w1"].ap(), T["w3"].ap(), T["w2"].ap()

    def load13(e_, jj):
        k_, t_, key_ = w13.next()
        w13.load(k_, t_[:, 0, :, :], w1d[e_, :, jj * 256:(jj + 1) * 256].rearrange("(k p) n -> p k n", p=128))
        w13.load(k_, t_[:, 1, :, :], w3d[e_, :, jj * 256:(jj + 1) * 256].rearrange("(k p) n -> p k n", p=128))
        return t_, key_

    pre13 = [load13(0, 0)]
    layer_norm(0)

    P.push()
    if moe:
        rw = small("rw", [128, 8, 8], T["rw"].ap().rearrange("(k p) e -> p k e", p=128), grp="small2")
        rb1 = small("rb1", [1, 8], T["rb"].ap(), grp="small2")
        sel8 = small("sel8", [8, 8, 128], T["sel8"].ap().rearrange("k (e m) -> k e m", m=128), grp="small2")
        ones32 = P.sb([1, 128], F32, name="ones32")
        P.op("vector", lambda e: e.memset(ones32[:], 1.0), writes=["ones32"])
        Lg = P.sb([128, NT // 128, 8], F32, name="Lg")
        gate = P.sb([128, NT // 128, 8], F32, name="gate")
        mx8 = P.sb([128, 8], F32, name="mx8")
        nm1 = P.sb([128, 1], F32, name="nm1")
        selm = P.sb([128, 8], F32, name="selm")
        ex = P.sb([128, 8], F32, name="ex")
        den = P.sb([128, 1], F32, name="den")
        gateT = P.sb([8, NT], F32, name="gateT")
        for ch in range(NT // 128):
            csl = slice(ch * 128, (ch + 1) * 128)
            tt = ch // 4
            for k in range(8):
                P.op("tensor", lambda e, k=k, csl=csl, ch=ch: e.matmul(PS[:, ch * 8:(ch + 1) * 8], lhsT=X32[:, k, csl], rhs=rw[:, k, :], start=(k == 0), stop=False),
                     reads=["small2", xk(k, tt)], writes=[bkey(0)])
            P.op("tensor", lambda e, ch=ch: e.matmul(PS[:, ch * 8:(ch + 1) * 8], lhsT=ones32[0:1, :], rhs=rb1[0:1, :], start=False, stop=True),
                 reads=["small2", "ones32"], writes=[bkey(0)])
        P.op("vector", lambda e: e.tensor_copy(Lg[:].rearrange("p c e -> p (c e)"), PS[:, 0:NT // 16]),
             reads=[bkey(0)], writes=["Lg"])
        for ch in range(NT // 128):
            P.op("vector", lambda e, ch=ch: e.max(mx8[:], Lg[:, ch, :]), reads=["Lg"], writes=["mx8"])
            P.op("vector", lambda e: e.tensor_scalar(nm1[:], mx8[:, 0:1], -1.0, None, ALU.mult), reads=["mx8"], writes=["nm1"])
            P.op("vector", lambda e, ch=ch: e.tensor_scalar(selm[:], Lg[:, ch, :], mx8[:, 1:2], None, ALU.is_ge), reads=["Lg", "mx8"], writes=["selm"])
            P.op("scalar", lambda e, ch=ch: e.activation(ex[:], Lg[:, ch, :], AF.Exp, bias=nm1[:]), reads=["Lg", "nm1"], writes=["ex"])
            P.op("vector", lambda e: e.tensor_tensor(ex[:], ex[:], selm[:], ALU.mult), reads=["ex", "selm"], writes=["ex"])
            P.op("vector", lambda e: e.reduce_sum(den[:], ex[:], axis=AX.X), reads=["ex"], writes=["den"])
            P.op("vector", lambda e: e.reciprocal(den[:], den[:]), reads=["den"], writes=["den"])
            P.op("vector", lambda e, ch=ch: e.tensor_scalar(gate[:, ch, :], ex[:], den[:], None, ALU.mult), reads=["ex", "den"], writes=[f"gate{ch}"])
            P.op("tensor", lambda e, ch=ch: e.transpose(PS[0:8, (1 + ch // 4) * 512 + (ch % 4) * 128:(1 + ch // 4) * 512 + (ch % 4 + 1) * 128], gate[:, ch, :], ident[:]),
                 reads=[f"gate{ch}", "small"], writes=[bkey(1 + ch // 4)])
        for tt in range(NTT):
            P.op("vector", lambda e, tt=tt: e.tensor_copy(gateT[:, tt * TT:(tt + 1) * TT], PS[0:8, (1 + tt) * 512:(2 + tt) * 512]),
                 reads=[bkey(1 + tt)], writes=[f"gateT{tt}"])
        gbc = P.sb([128, 2 * TT], F32, name="gbc")

    G = NT
    act = P.sb([128, NJ, G], BF16, name="act")
    w2s = WStream(P, "w2s", [128, NJ, 128], nbuf=2)
    sil = [P.sb([128, TT], F32, name=f"sil{i}") for i in range(2)]
    ftmp = [P.sb([128, TT], F32, name=f"ftmp{i}") for i in range(2)]


    def load2(e_, m):
        k_, t_, key_ = w2s.next()
        w2s.load(k_, t_[:], w2d[e_, :, m * 128:(m + 1) * 128].rearrange("(j p) n -> p j n", p=128))
        return t_, key_

    hb = 0
    si = 0
    first_scale_done = set()
    for tg in range(1):
        for e_ in range(E):
            if moe:
                for h in range(2):
                    tt = tg * 2 + h
                    P.op("tensor", lambda e, e_=e_, tt=tt, h=h: e.matmul(bank(6 + h), lhsT=sel8[:, e_, :], rhs=gateT[:, tt * TT:(tt + 1) * TT], start=True, stop=True),
                         reads=["small2", f"gateT{tt}"], writes=[bkey(6 + h)])
                    P.op("scalar", lambda e, h=h: e.copy(gbc[:, h * TT:(h + 1) * TT], bank(6 + h)), reads=[bkey(6 + h)], writes=[f"gbc{h}"])
            nx13 = pre13.pop() if pre13 else load13(e_, 0)
            for jj in range(NJ // 2):
                w_t, wkey = nx13
                if jj + 1 < NJ // 2:
                    nx13 = load13(e_, jj + 1)
                for jh in range(2):
                    j = jj * 2 + jh
                    for h in range(2):
                        tt = tg * 2 + h
                        tsl = slice(tt * TT, (tt + 1) * TT)
                        b1 = (hb % 3) * 2
                        b3 = b1 + 1
                        hb += 1
                        for k in range(8):
                            P.op("tensor", lambda e, b1=b1, k=k, jh=jh, tsl=tsl, w_t=w_t: e.matmul(bank(b1), lhsT=w_t[:, 0, k, jh * 128:(jh + 1) * 128], rhs=Xbf[:, k, tsl], start=(k == 0), stop=(k == 7)),
                                 reads=[wkey, xbk(k, tt)], writes=[bkey(b1)])
                        for k in range(8):
                            P.op("tensor", lambda e, b3=b3, k=k, jh=jh, tsl=tsl, w_t=w_t: e.matmul(bank(b3), lhsT=w_t[:, 1, k, jh * 128:(jh + 1) * 128], rhs=Xbf[:, k, tsl], start=(k == 0), stop=(k == 7)),
                                 reads=[wkey, xbk(k, tt)], writes=[bkey(b3)])
                        s_ = sil[si % 2]
                        sk = f"sil{si % 2}"
                        si += 1
                        P.op("scalar", lambda e, b1=b1, s_=s_: e.activation(s_[:], bank(b1), AF.Silu), reads=[bkey(b1)], writes=[sk])
                        P.op("vector", lambda e, b3=b3, s_=s_, j=j, h=h: e.tensor_tensor(act[:, j, h * TT:(h + 1) * TT], s_[:], bank(b3), ALU.mult),
                             reads=[sk, bkey(b3)], writes=[f"act{j}_{h}"])
            nx2 = load2(e_, 0)
            for m in range(8):
                w_t, wkey = nx2
                if m + 1 < 8:
                    nx2 = load2(e_, m + 1)
                for h in range(2):
                    tt = tg * 2 + h
                    tsl = slice(tt * TT, (tt + 1) * TT)
                    b = (hb % 3) * 2
                    hb += 1
                    for j in range(NJ):
                        P.op("tensor", lambda e, b=b, j=j, h=h, w_t=w_t: e.matmul(bank(b), lhsT=w_t[:, j, :], rhs=act[:, j, h * TT:(h + 1) * TT], start=(j == 0), stop=(j == NJ - 1)),
                             reads=[wkey, f"act{j}_{h}"], writes=[bkey(b)])
                    if not moe:
                        P.op("vector", lambda e, b=b, m=m, tsl=tsl: e.scalar_tensor_tensor(X32[:, m, tsl], X32[:, m, tsl], ALPHA, bank(b), ALU.mult, ALU.add),
                             reads=[bkey(b), xk(m, tt)], writes=[xk(m, tt)])
                    else:
                        f_ = ftmp[si % 2]
                        fk = f"ftmp{si % 2}"
                        si += 1
                        P.op("vector", lambda e, b=b, f_=f_, h=h: e.tensor_tensor(f_[:], bank(b), gbc[:, h * TT:(h + 1) * TT], ALU.mult),
                             reads=[bkey(b), f"gbc{h}"], writes=[fk])
                        if (m, tt) not in first_scale_done:
                            first_scale_done.add((m, tt))
                            P.op("vector", lambda e, f_=f_, m=m, tsl=tsl: e.scalar_tensor_tensor(X32[:, m, tsl], X32[:, m, tsl], ALPHA, f_[:], ALU.mult, ALU.add),
                                 reads=[fk, xk(m, tt)], writes=[xk(m, tt)])
                        else:
                            P.op("vector", lambda e, f_=f_, m=m, tsl=tsl: e.tensor_tensor(X32[:, m, tsl], X32[:, m, tsl], f_[:], ALU.add),
                                 reads=[fk, xk(m, tt)], writes=[xk(m, tt)])

    P.pop()
    layer_norm(1, write_bf=(T.get("xob") is not None), bbase=4)

    xo = T["xo"].ap()[:, hs].rearrange("(c p) n -> p c n", p=128)
    for c in range(8):
        P.op("sync", lambda e, c=c: e.dma_start(out=xo[:, c, :], in_=X32[:, c, :]), reads=[xk(c, t) for t in range(NTT)], writes=["out_x"], dma_key="outx")
        if T.get("xob") is not None:
            xb_t = T["xob"][c // 4].ap()
            P.op("sync", lambda e, c=c, xb_t=xb_t: e.dma_start(out=xb_t[(c % 4) * 128:(c % 4 + 1) * 128, hs], in_=Xbf[:, c, :]), reads=[xbk(c, t) for t in range(NTT)], writes=["out_xb"], dma_key="outx")
    P.wait_res("sync", ["out_x", "out_xb"])
    P.pop()


SUB = 9

S = 4096
TT = 512
NTA = S // TT
DILS = (1, 4, 16)
NA = 2692
LNSC = math.log(192 ** -0.5)
TWO_PI = 2.0 * math.pi
CW1 = 6.28125
CW2 = TWO_PI - CW1
MAGIC = 12582912.0


def build_A(nc, P, T, tag="A", stage=99):
    P.push()
    P.prefix = f"{tag}_"
    PS = P.ps([128, 4096], F32, name="psA")
    bank = lambda i: PS[:, i * 512:(i + 1) * 512]
    bkey = lambda i: f"psA{i}"

    def small(name, shape, src, dtype=F32, eng="sync", grp="smallA"):
        t = P.sb(shape, dtype, name=name)
        if eng == "gpsimd":
            grp = grp + "_g"
        P.op(eng, lambda e: e.dma_start(out=t[:], in_=src), writes=[grp], dma_key=grp)
        return t

    xTa = P.sb([128, 8, S], BF16, name="xTa")
    if T["xsrc"][0] == "ext":
        xsrc = T["xsrc"][1].ap().rearrange("(c p) n -> p c n", p=128)
        for tt in range(NTA):
            tsl = slice(tt * TT, (tt + 1) * TT)
            P.op("gpsimd", lambda e, tsl=tsl: e.dma_start(out=xTa[:, :, tsl], in_=xsrc[:, :, tsl]), writes=[f"xTa{tt}"], dma_key=f"xTa{tt % 4}")
    else:
        for r in range(2):
            for hq in range(4):
                tt = r * 4 + hq
                for i in range(2):
                    xg = T["xsrc"][1 + i].ap()
                    P.op("sync", lambda e, r=r, hq=hq, i=i, xg=xg, tt=tt: e.dma_start(out=xTa[:, i * 4:(i + 1) * 4, tt * TT:(tt + 1) * TT], in_=xg[r * 512:(r + 1) * 512, hq * TT:(hq + 1) * TT].rearrange("(c p) n -> p c n", p=128)),
                         reads=["xg"], writes=[f"xTa{tt}"], dma_key=f"xTa_h{tt % 4}")

    ident = small("ident", [128, 128], T["ident"].ap())
    triU = small("triU", [128, 128], T["triU"].ap())
    permM = small("permM", [128, 128], T["permM"].ap())
    invf = small("invf", [128, 1], T["invf"].ap())
    sgn = small("sgn", [128, 1], T["sgn"].ap())
    maskA = small("maskA", [128, 512], T["maskA"].ap(), dtype=BF16, eng="gpsimd")
    bqk = small("bqk", [128, 6], T["bqk"].ap())
    bva = small("bva", [128, 384], T["bva"].ap().partition_broadcast(128))
    bml = small("bml", [128, 8], T["bml"].ap())
    bvm = small("bvm", [128, 384], T["bvm"].ap().partition_broadcast(128))
    bif = small("bif", [128, 4], T["bif"].ap().partition_broadcast(128))
    cw = small("cw", [128, 8, 4], T["cw"].ap())
    cb = small("cb", [128, 8], T["cb"].ap())
    bo = small("bo", [128, 6], T["bo"].ap())
    ones_bf = P.sb([128, 128], BF16, name="ones_bf")
    P.op("vector", lambda e: e.memset(ones_bf[:], 1.0), writes=["ones_bf"])
    ones_f = P.sb([128, 128], F32, name="ones_f")
    P.op("vector", lambda e: e.memset(ones_f[:], 1.0), writes=["ones_f"])
    lnsc = P.sb([128, 1], F32, name="lnsc")
    P.op("vector", lambda e: e.memset(lnsc[:], LNSC), writes=["lnsc"])

    wA = T["wA"].ap()

    P.push()
    cosT = P.sb([128, S], F32, name="cosT")
    sinT = P.sb([128, S], F32, name="sinT")
    P.push()
    posi = P.sb([128, S], I32, name="posi")
    P.op("sync", lambda e: e.dma_start(out=posi[:], in_=T["pos"].ap().partition_broadcast(128)), writes=["posi"], dma_key="posi")
    ang = P.sb([128, S], F32, name="ang")
    kk = P.sb([128, S], F32, name="kk")
    rr = P.sb([128, S], F32, name="rr")
    P.op("vector", lambda e: e.tensor_copy(ang[:], posi[:]), reads=["posi"], writes=["ang"])
    P.op("vector", lambda e: e.tensor_scalar(ang[:], ang[:], invf[:], None, ALU.mult), reads=["ang", "smallA"], writes=["ang"])
    P.op("vector", lambda e: e.tensor_scalar(kk[:], ang[:], 1.0 / TWO_PI, MAGIC, ALU.mult, ALU.add), reads=["ang"], writes=["kk"])
    P.op("vector", lambda e: e.tensor_scalar(kk[:], kk[:], MAGIC, None, ALU.subtract), reads=["kk"], writes=["kk"])
    P.op("vector", lambda e: e.scalar_tensor_tensor(rr[:], kk[:], -CW1, ang[:], ALU.mult, ALU.add), reads=["kk", "ang"], writes=["rr"])
    P.op("vector", lambda e: e.scalar_tensor_tensor(rr[:], kk[:], -CW2, rr[:], ALU.mult, ALU.add), reads=["kk", "rr"], writes=["rr"])
    P.op("vector", lambda e: e.tensor_scalar(rr[:], rr[:], math.pi, -math.pi, ALU.min, ALU.max), reads=["rr"], writes=["rr"])
    P.op("scalar", lambda e: e.activation(sinT[:], rr[:], AF.Sin, scale=sgn[:]), reads=["rr", "smallA"], writes=["sinT"])
    P.op("vector", lambda e: e.tensor_scalar(ang[:], rr[:], math.pi / 2, None, ALU.add), reads=["rr"], writes=["ang"])
    P.op("vector", lambda e: e.tensor_scalar(kk[:], ang[:], math.pi, None, ALU.is_gt), reads=["ang"], writes=["kk"])
    P.op("vector", lambda e: e.scalar_tensor_tensor(ang[:], kk[:], -TWO_PI, ang[:], ALU.mult, ALU.add), reads=["kk", "ang"], writes=["ang"])
    P.op("vector", lambda e: e.tensor_scalar(ang[:], ang[:], math.pi, -math.pi, ALU.min, ALU.max), reads=["ang"], writes=["ang"])
    P.op("scalar", lambda e: e.activation(cosT[:], ang[:], AF.Sin), reads=["ang"], writes=["cosT"])
    P.pop(dma=False)
    if stage == 1:
        P.op("sync", lambda e: e.dma_start(out=T["dbg"].ap()[:, 0:S], in_=cosT[:]), reads=["cosT"], writes=["dbgo"], dma_key="dbg")
        P.op("sync", lambda e: e.dma_start(out=T["dbg"].ap()[:, S:2 * S], in_=sinT[:]), reads=["sinT"], writes=["dbgo"], dma_key="dbg")
        P.wait_res("sync", ["dbgo"])
        P.pop(); P.pop()
        return

    Y = P.sb([128, 2, S], F32, name="Y")
    QK = [P.sb([128, 2, S], BF16, name=f"QK{i}") for i in range(2)]
    V = [P.sb([128, 32, 128], BF16, name=f"V{i}") for i in range(2)]
    wAt = WStream(P, "wAt", [128, 8, 384], nbuf=2)
    qf = [P.sb([128, TT], F32, name=f"qf{i}") for i in range(2)]
    t1 = [P.sb([128, TT], F32, name=f"t1{i}") for i in range(1)] * 2
    t2 = [P.sb([128, TT], F32, name=f"t2{i}") for i in range(1)] * 2
    pT = [P.sb([128, 512], BF16, name=f"pT{i}") for i in range(2)]
    cnt_a = {"ci": 0}
    B_PROJ, B_SW, B_V, B_PV = 0, 1, 1, (2, 3)

    def make_group(g, dil):
        gp = g % 2
        kq, w_t, wkey = wAt.next()
        QKg, Vg = QK[gp], V[gp]
        qkk = lambda which, tt: f"QK{gp}_{which}_{tt}"
        span = 128 * dil
        blocks = [(n, r) for n in range(S // span) for r in range(dil)]
        tok = lambda n, r: slice(n * span + r, n * span + r + span - dil + 1, dil)
        tiles_of = lambda n, r: sorted(set(range((n * span) // TT, (n * span + span - 1) // TT + 1)))
        units = []

        def load_w():
            for k in range(8):
                wAt.load(kq, w_t[:, k, :], wA[k * 128:(k + 1) * 128, g * 384:(g + 1) * 384])

        def qk_unit(tt, which):
            tsl = slice(tt * TT, (tt + 1) * TT)
            ci = cnt_a["ci"]
            cnt_a["ci"] += 1
            q_, qk_ = qf[ci % 2], f"qf{ci % 2}"
            a_, ak_ = t1[0], "t1_0"
            b_, bk_ = t2[0], "t2_0"
            for k in range(8):
                P.op("tensor", lambda e, k=k: e.matmul(bank(B_PROJ), lhsT=w_t[:, k, which * 128:(which + 1) * 128], rhs=xTa[:, k, tsl], start=(k == 0), stop=(k == 7)),
                     reads=[wkey, f"xTa{tt}"], writes=[bkey(B_PROJ)])
            col = 2 * g + which
            P.op("scalar", lambda e: e.activation(q_[:], bank(B_PROJ), AF.Identity, bias=bqk[:, col:col + 1]),
                 reads=[bkey(B_PROJ), "smallA"], writes=[qk_])
            P.op("tensor", lambda e: e.matmul(bank(B_SW), lhsT=permM[:], rhs=q_[:], start=True, stop=True),
                 reads=[qk_, "smallA"], writes=[bkey(B_SW)])
            P.op("vector", lambda e: e.tensor_tensor(a_[:], q_[:], cosT[:, tsl], ALU.mult), reads=[qk_, "cosT"], writes=[ak_])
            P.op("vector", lambda e: e.tensor_tensor(b_[:], bank(B_SW), sinT[:, tsl], ALU.mult), reads=[bkey(B_SW), "sinT"], writes=[bk_])
            P.op("vector", lambda e: e.tensor_tensor(QKg[:, which, tsl], a_[:], b_[:], ALU.add), reads=[ak_, bk_], writes=[qkk(which, tt)])

        def v_unit(bi4):
            for q4 in range(4):
                n, r = blocks[bi4 + q4]
                for k in range(8):
                    P.op("tensor", lambda e, k=k, q4=q4, tk=tok(n, r): e.matmul(PS[:, B_V * 512 + q4 * 128:B_V * 512 + (q4 + 1) * 128], lhsT=xTa[:, k, tk], rhs=w_t[:, k, 256:384], start=(k == 0), stop=(k == 7)),
                         reads=[wkey] + [f"xTa{t_}" for t_ in tiles_of(n, r)], writes=[bkey(B_V)])
            for q4 in range(4):
                P.op("vector", lambda e, q4=q4: e.tensor_tensor(Vg[:, bi4 + q4, :], PS[:, B_V * 512 + q4 * 128:B_V * 512 + (q4 + 1) * 128], bva[:, g * 128:(g + 1) * 128], ALU.add),
                     reads=[bkey(B_V), "smallA"], writes=[f"V{gp}_{bi4 + q4}"])

        units.append(load_w)
        qk_units = [(lambda tt=tt, which=which: qk_unit(tt, which)) for tt in range(NTA) for which in range(2)]
        v_units = [(lambda bi4=bi4: v_unit(bi4)) for bi4 in range(0, 32, 4)]
        if g == 0:
            units += v_units + qk_units
        else:
            units += qk_units + v_units

        def blk_setup(bi, n, r):
            bs = 4 + 2 * (bi % 2)
            bp = B_PV[bi % 2]
            p_ = pT[bi % 2]
            pk_ = f"pT{bi % 2}"
            kbs = [(n, 0)] + ([(n - 1, 1)] if n >= 1 else [])
            ncol = 128 * len(kbs)
            qreads = [qkk(0, t_) for t_ in tiles_of(n, r)]
            return bs, bp, p_, pk_, kbs, ncol, qreads

        def front(bi):
            n, r = blocks[bi]
            bs, bp, p_, pk_, kbs, ncol, qreads = blk_setup(bi, n, r)
            for (kn, slot) in kbs:
                kreads = [qkk(1, t_) for t_ in tiles_of(kn, r)]
                for hd in range(2):
                    c0 = (bs + hd) * 512 + slot * 128
                    P.op("tensor", lambda e, c0=c0, hd=hd, tkk=tok(kn, r), tkq=tok(n, r): e.matmul(PS[:, c0:c0 + 128], lhsT=QKg[hd * 64:(hd + 1) * 64, 1, tkk], rhs=QKg[hd * 64:(hd + 1) * 64, 0, tkq], start=True, stop=True),
                         reads=qreads + kreads, writes=[bkey(bs + hd)])
            for hd in range(2):
                P.op("scalar", lambda e, hd=hd: e.activation(p_[:, hd * 256:hd * 256 + ncol], PS[:, (bs + hd) * 512:(bs + hd) * 512 + ncol], AF.Exp, scale=0.125),
                     reads=[bkey(bs + hd)], writes=[pk_ + f"h{hd}"])
                P.op("vector", lambda e, hd=hd: e.tensor_tensor(p_[:, hd * 256:hd * 256 + ncol], p_[:, hd * 256:hd * 256 + ncol], maskA[:, 0:ncol], ALU.mult),
                     reads=[pk_ + f"h{hd}", "smallA_g"], writes=[pk_ + f"h{hd}"])

        def back(bi):
            n, r = blocks[bi]
            bs, bp, p_, pk_, kbs, ncol, qreads = blk_setup(bi, n, r)
            for hd in range(2):
                for oi in range(2):
                    c0 = bp * 512 + hd * 256 + oi * 128
                    for ki, (kn, slot) in enumerate(kbs):
                        kblk = blocks.index((kn, r))
                        rhs_c = hd * 256 + slot * 128
                        if oi == 0:
                            P.op("tensor", lambda e, c0=c0, kblk=kblk, rhs_c=rhs_c, ki=ki, nk=len(kbs): e.matmul(PS[:, c0:c0 + 128], lhsT=Vg[:, kblk, :], rhs=p_[:, rhs_c:rhs_c + 128], start=(ki == 0), stop=(ki == nk - 1)),
                                 reads=[pk_ + f"h{hd}", f"V{gp}_{kblk}"], writes=[bkey(bp)])
                        else:
                            P.op("tensor", lambda e, c0=c0, rhs_c=rhs_c, ki=ki, nk=len(kbs): e.matmul(PS[:, c0:c0 + 128], lhsT=ones_bf[:], rhs=p_[:, rhs_c:rhs_c + 128], start=(ki == 0), stop=(ki == nk - 1)),
                                 reads=[pk_ + f"h{hd}", "ones_bf"], writes=[bkey(bp)])
            for hd in range(2):
                rs = slice(hd * 64, (hd + 1) * 64)
                src = PS[rs, bp * 512 + hd * 256:bp * 512 + hd * 256 + 256].rearrange("p (a q) -> p a q", a=2)
                ykeys = [f"Y{hd}_{t_}" for t_ in tiles_of(n, r)]
                if g == 0:
                    P.op("vector", lambda e, rs=rs, src=src, tkq=tok(n, r): e.tensor_copy(Y[rs, :, tkq], src),
                         reads=[bkey(bp)], writes=ykeys)
                else:
                    P.op("vector", lambda e, rs=rs, src=src, tkq=tok(n, r): e.tensor_tensor(Y[rs, :, tkq], Y[rs, :, tkq], src, ALU.add),
                         reads=[bkey(bp)] + ykeys, writes=ykeys)

        return units, len(blocks), front, back

    groups = [make_group(g, dil) for g, dil in enumerate(DILS)]
    for u in groups[0][0]:
        u()
    for g in range(len(DILS)):
        units, nblk, front, back = groups[g]
        nxt_units = list(groups[g + 1][0]) if g + 1 < len(DILS) else []
        front(0)
        for bi in range(nblk):
            if bi + 1 < nblk:
                front(bi + 1)
            back(bi)
            if nxt_units:
                nxt_units.pop(0)()
        for u in nxt_units:
            u()

    yao = [P.sb([128, TT], BF16, name=f"yao{i}") for i in range(2)]
    for tt in (range(NTA) if stage not in (20, 21, 24, 25) else []):
        tsl = slice(tt * TT, (tt + 1) * TT)
        yt_ = yao[tt % 2]
        P.op("vector", lambda e, tsl=tsl: e.reciprocal(Y[:, 1, tsl], Y[:, 1, tsl]), reads=[f"Y0_{tt}", f"Y1_{tt}"], writes=[f"Yd_{tt}"])
        P.op("vector", lambda e, tsl=tsl, yt_=yt_: e.tensor_tensor(yt_[:], Y[:, 0, tsl], Y[:, 1, tsl], ALU.mult), reads=[f"Yd_{tt}", f"Y0_{tt}", f"Y1_{tt}"], writes=[f"yao{tt % 2}"])
        P.op("sync", lambda e, tt=tt, yt_=yt_: e.dma_start(out=T["ysend"][tt // 4].ap()[0:128, (tt % 4) * TT:(tt % 4 + 1) * TT], in_=yt_[:]), reads=[f"yao{tt % 2}"], writes=["out_ya"], dma_key="outA")
    P.wait_res("sync", ["out_ya"])
    P.pop()

    if stage == 2 or 20 <= stage <= 25:
        P.pop()
        return
    if T.get("mid_hook") is not None:
        T["mid_hook"]()
    P.push()
    PROJ, VIF, GB, OB, DB = 0, 1, (2, 3), (4, 5), (6, 7)
    wMl = WStream(P, "wMl", [128, 8, 1540], nbuf=1)
    km, wm, wmk = wMl.next()
    for k in range(8):
        wMl.load(km, wm[:, k, :], wA[k * 128:(k + 1) * 128, 1152:2692])
    PW = [128, 64, 128, 64]
    POFF = [0, 128, 192, 320]
    cbuf = P.sb([128, 8, 3 + TT], BF16, name="cbuf")
    P.op("vector", lambda e: e.memset(cbuf[:], 0.0), writes=[f"cbuf{i}" for i in range(8)])
    Dg = P.sb([128, 8, 4, 128], BF16, name="Dg")
    for pid_ in range(8):
        for j_ in range(4):
            P.op("vector", lambda e, pid_=pid_, j_=j_: e.tensor_scalar(Dg[:, pid_, j_, :], ident[:], cw[:, pid_, j_:j_ + 1], None, ALU.mult),
                 reads=["smallA"], writes=[f"Dg{pid_}"])
    QKm = [P.sb([128, 8, TT], BF16, name=f"QKm{i}") for i in range(2)]
    K32 = [P.sb([128, 4, TT], F32, name=f"K32{i}") for i in range(2)]
    og = [P.sb([128, 6, TT], BF16, name=f"og{i}") for i in range(2)]
    vaug = [P.sb([128, 4, 2, 256], BF16, name=f"vaug{i}") for i in range(2)]
    for i in range(2):
        P.op("vector", lambda e, i=i: e.memset(vaug[i][:, :, :, 192:256], 1.0), writes=[f"vaug{i}"])
    ifs = [P.sb([128, 4, 4], F32, name=f"ifs{i}") for i in range(2)]
    lpos = [P.sb([128, 4, 2], F32, name=f"lpos{i}") for i in range(2)]
    C32 = [[P.sb([128, 256], F32, name=f"C32_{h}{ab}") for ab in "AB"] for h in range(2)]
    Cb = [[P.sb([128, 256], BF16, name=f"Cb_{h}{ab}") for ab in "AB"] for h in range(2)]
    for h in range(2):
        for ab in range(2):
            P.op("vector", lambda e, h=h, ab=ab: e.memset(C32[h][ab][:], 0.0), writes=[f"C32_{h}{ab}"])
            P.op("vector", lambda e, h=h, ab=ab: e.memset(Cb[h][ab][:], 0.0), writes=[f"Cb_{h}{ab}"])
    NEGm = P.sb([128, 128], F32, name="NEGm")
    P.op("vector", lambda e: e.tensor_scalar(NEGm[:], triU[:], -1.0, 1.0e4, ALU.add, ALU.mult), reads=["smallA"], writes=["NEGm"])
    NEG2 = P.sb([128, 2, 128], F32, name="NEG2")
    for h in range(2):
        P.op("vector", lambda e, h=h: e.tensor_copy(NEG2[:, h, :], NEGm[:]), reads=["NEGm"], writes=["NEG2"])
    LFb2 = P.sb([128, 2, 128], F32, name="LFb2")
    acolT = P.sb([128, 4], F32, name="acolT")
    wpre2 = P.sb([128, 2], F32, name="wpre2")
    DTa2 = P.sb([128, 2, 128], F32, name="DTa2")
    DT2 = P.sb([128, 2, 128], F32, name="DT2")
    Erow2 = P.sb([128, 2, 128], F32, name="Erow2")
    dbl = lambda nm, shape, dt: [P.sb(shape, dt, name=f"{nm}_{c2}") for c2 in range(2)]
    wcol2 = dbl("wcol2", [128, 2], F32)
    dec2 = dbl("dec2", [128, 2], F32)
    scT2 = dbl("scT2", [128, 2, 128], BF16)
    qsA2 = dbl("qsA2", [128, 2, 128], BF16)
    qsB2 = dbl("qsB2", [128, 2, 128], BF16)
    kw2 = dbl("kw2", [128, 2, 192], BF16)
    rden2 = P.sb([128, 2, 128], F32, name="rden2")
    ogr2 = P.sb([128, 3, 2, 128], F32, name="ogr2")
    yco2 = P.sb([128, 2, 3, TT], BF16, name="yco2")
    cnt = {"pi": 0}

    def proj_groups(tt):
        tsl = slice(tt * TT, (tt + 1) * TT)
        par = tt % 2
        QKt, K32t, ogt, vat, ift, lpt = QKm[par], K32[par], og[par], vaug[par], ifs[par], lpos[par]
        groups = []

        def qk_piece(which, pc):
            w_, o_ = PW[pc], which * 384 + POFF[pc]
            pid = which * 4 + pc
            cnt["pi"] += 1
            pb = (PROJ, VIF)[cnt["pi"] % 2]
            for k in range(8):
                P.op("tensor", lambda e, k=k: e.matmul(PS[0:w_, pb * 512:(pb + 1) * 512], lhsT=wm[:, k, o_:o_ + w_], rhs=xTa[:, k, tsl], start=(k == 0), stop=(k == 7)),
                     reads=[wmk, f"xTa{tt}"], writes=[bkey(pb)])
            ck = f"cbuf{pid}"
            P.op("vector", lambda e: e.tensor_copy(cbuf[0:w_, pid, 0:3], cbuf[0:w_, pid, TT:TT + 3]), reads=[ck], writes=[ck])
            P.op("scalar", lambda e: e.activation(cbuf[0:w_, pid, 3:3 + TT], PS[0:w_, pb * 512:(pb + 1) * 512], AF.Identity, bias=bml[0:w_, pid:pid + 1]),
                 reads=[bkey(pb), "smallA", ck], writes=[ck])
            for j in range(4):
                P.op("tensor", lambda e, j=j: e.matmul(PS[0:w_, pb * 512:(pb + 1) * 512], lhsT=Dg[0:w_, pid, j, 0:w_], rhs=cbuf[0:w_, pid, j:j + TT], start=(j == 0), stop=(j == 3)),
                     reads=[ck, f"Dg{pid}"], writes=[bkey(pb)])
            P.op("scalar", lambda e: e.activation(QKt[0:w_, pid, :], PS[0:w_, pb * 512:(pb + 1) * 512], AF.Silu, bias=cb[0:w_, pid:pid + 1]),
                 reads=[bkey(pb), "smallA"], writes=[f"QKm{par}_{pid}"])
            if which == 1:
                P.op("scalar", lambda e: e.activation(K32t[0:w_, pc, :], PS[0:w_, pb * 512:(pb + 1) * 512], AF.Silu, bias=cb[0:w_, pid:pid + 1]),
                     reads=[bkey(pb), "smallA"], writes=[f"K32{par}_{pc}"])

        def og_piece(q6):
            o_ = 1152 + q6 * 64
            for k in range(8):
                P.op("tensor", lambda e, k=k: e.matmul(PS[0:64, PROJ * 512:(PROJ + 1) * 512], lhsT=wm[:, k, o_:o_ + 64], rhs=xTa[:, k, tsl], start=(k == 0), stop=(k == 7)),
                     reads=[wmk, f"xTa{tt}"], writes=[bkey(PROJ)])
            P.op("scalar", lambda e: e.activation(ogt[0:64, q6, :], PS[0:64, PROJ * 512:(PROJ + 1) * 512], AF.Sigmoid, bias=bo[0:64, q6:q6 + 1]),
                 reads=[bkey(PROJ), "smallA"], writes=[f"og{par}_{q6}"])

        def v_if(cc):
            csl = slice(tt * TT + cc * 128, tt * TT + (cc + 1) * 128)
            for k in range(8):
                P.op("tensor", lambda e, k=k: e.matmul(PS[:, VIF * 512:VIF * 512 + 384], lhsT=xTa[:, k, csl], rhs=wm[:, k, 768:1152], start=(k == 0), stop=(k == 7)),
                     reads=[wmk, f"xTa{tt}"], writes=[bkey(VIF)])
            for k in range(8):
                P.op("tensor", lambda e, k=k: e.matmul(PS[:, VIF * 512 + 384:VIF * 512 + 388], lhsT=xTa[:, k, csl], rhs=wm[:, k, 1536:1540], start=(k == 0), stop=(k == 7)),
                     reads=[wmk, f"xTa{tt}"], writes=[bkey(VIF)])
            P.op("vector", lambda e: e.tensor_tensor(vat[:, cc, :, 0:192], PS[:, VIF * 512:VIF * 512 + 384].rearrange("p (h d) -> p h d", h=2), bvm[:].rearrange("p (h d) -> p h d", h=2), ALU.add),
                 reads=[bkey(VIF), "smallA", f"vaug{par}"], writes=[f"vaug{par}_{cc}"])
            P.op("vector", lambda e: e.tensor_tensor(ift[:, cc, :], PS[:, VIF * 512 + 384:VIF * 512 + 388], bif[:], ALU.add),
                 reads=[bkey(VIF), "smallA"], writes=[f"ifs{par}_{cc}"])

        def lpos_grp():
            P.op("scalar", lambda e: e.activation(lpt[:], ift[:, :, 2:4], AF.Exp, scale=-1.0), reads=[f"ifs{par}_{c_}" for c_ in range(4)], writes=[f"lpos{par}"])
            P.op("scalar", lambda e: e.activation(lpt[:], lpt[:], AF.Ln, bias=1.0), reads=[f"lpos{par}"], writes=[f"lpos{par}"])

        for cc in range(4):
            groups.append(lambda cc=cc: v_if(cc))
        groups.append(lpos_grp)
        for which in range(2):
            for pc in range(4):
                groups.append(lambda which=which, pc=pc: qk_piece(which, pc))
        for q6 in range(6):
            groups.append(lambda q6=q6: og_piece(q6))
        return groups

    GA, GBk = 2, 3

    def sub_step(tt, cc, sub):
        par = tt % 2
        c2 = cc % 2
        QKt, K32t, ogt, vat, ift, lpt = QKm[par], K32[par], og[par], vaug[par], ifs[par], lpos[par]
        csl = slice(cc * 128, (cc + 1) * 128)
        ga, gb = bkey(GA), bkey(GBk)
        brow2 = PS[:, GA * 512:GA * 512 + 256]
        st2 = PS[:, GA * 512 + 256:GA * 512 + 512]
        bL = PS[:, GA * 512 + 127:GA * 512 + 256:128]
        bcol = PS[:, GBk * 512 + 384:GBk * 512 + 386]
        v3 = lambda ap: ap.rearrange("p (h c) -> p h c", h=2)
        allq = [f"QKm{par}_{i}" for i in range(4)]
        allk = [f"QKm{par}_{i}" for i in range(4, 8)]
        if sub == 0:
            for h in range(2):
                P.op("vector", lambda e, h=h: e.tensor_scalar(LFb2[:, h, :], ones_f[:], lpt[:, cc, h:h + 1], None, ALU.mult), reads=[f"lpos{par}", "ones_f"], writes=["LFb2"])
            for h in range(2):
                P.op("tensor", lambda e, h=h: e.matmul(PS[:, GA * 512 + h * 128:GA * 512 + (h + 1) * 128], lhsT=LFb2[:, h, :], rhs=triU[:], start=True, stop=True), reads=["LFb2", "smallA"], writes=[ga])
            for h in range(2):
                kA, kB = QKt[:, 4 + 2 * h, csl], QKt[0:64, 4 + 2 * h + 1, csl]
                qA, qB = QKt[:, 2 * h, csl], QKt[0:64, 2 * h + 1, csl]
                st = PS[:, GA * 512 + 256 + h * 128:GA * 512 + 256 + (h + 1) * 128]
                P.op("tensor", lambda e, st=st, kA=kA, qA=qA: e.matmul(st, lhsT=kA, rhs=qA, start=True, stop=False), reads=allq + allk, writes=[ga])
                P.op("tensor", lambda e, st=st, kB=kB, qB=qB: e.matmul(st, lhsT=kB, rhs=qB, start=False, stop=True), reads=allq + allk, writes=[ga])
            P.op("tensor", lambda e: e.matmul(bcol, lhsT=triU[:], rhs=lpt[:, cc, :], start=True, stop=True), reads=[f"lpos{par}", "smallA"], writes=[gb])
            for h in range(2):
                ktp = PS[:, GBk * 512 + h * 192:GBk * 512 + (h + 1) * 192]
                P.op("tensor", lambda e, ktp=ktp, h=h: e.transpose(ktp[:, 0:128], K32t[:, 2 * h, csl], ident[:]), reads=[f"K32{par}_{2 * h}", "smallA"], writes=[gb])
                P.op("tensor", lambda e, ktp=ktp, h=h: e.transpose(ktp[:, 128:192], K32t[0:64, 2 * h + 1, csl], ident[0:64, 0:64]), reads=[f"K32{par}_{2 * h + 1}", "smallA"], writes=[gb])
        elif sub == 1:
            P.op("vector", lambda e: e.tensor_tensor(acolT[:, 0:2], ift[:, cc, 0:2], bcol, ALU.add), reads=[f"ifs{par}_{cc}", gb], writes=["acol0"])
            P.op("vector", lambda e: e.tensor_tensor(wpre2[:], acolT[:, 0:2], bL, ALU.subtract), reads=["acol0", ga], writes=["wpre2"])
            P.op("vector", lambda e: e.tensor_scalar(acolT[:, 2:4], acolT[:, 0:2], LNSC, None, ALU.add), reads=["acol0"], writes=["acol1"])
            P.op("vector", lambda e: e.scalar_tensor_tensor(DTa2[:], v3(brow2), -1.0, NEG2[:], ALU.mult, ALU.add), reads=[ga, "NEG2"], writes=["DTa2"])
        elif sub == 2:
            P.op("scalar", lambda e: e.activation(wcol2[c2][:], wpre2[:], AF.Exp), reads=["wpre2"], writes=[f"wcol2_{c2}"])
            P.op("scalar", lambda e: e.activation(dec2[c2][:], bL, AF.Exp, scale=-1.0), reads=[ga], writes=[f"dec2_{c2}"])
            P.op("scalar", lambda e: e.activation(Erow2[:], v3(brow2), AF.Exp, scale=-1.0, bias=lnsc[:]), reads=[ga, "lnsc"], writes=["Erow2"])
            for h in range(2):
                P.op("scalar", lambda e, h=h: e.activation(DT2[:, h, :], DTa2[:, h, :], AF.Exp, bias=acolT[:, 2 + h:3 + h]), reads=["DTa2", "acol1"], writes=["DT2"])
        elif sub == 3:
            P.op("vector", lambda e: e.tensor_tensor(scT2[c2][:], v3(st2), DT2[:], ALU.mult), reads=[ga, "DT2"], writes=[f"scT2_{c2}"])
            for h in range(2):
                ktp = PS[:, GBk * 512 + h * 192:GBk * 512 + (h + 1) * 192]
                P.op("vector", lambda e, h=h, ktp=ktp: e.tensor_scalar(kw2[c2][:, h, :], ktp, wcol2[c2][:, h:h + 1], None, ALU.mult), reads=[gb, f"wcol2_{c2}"], writes=[f"kw2_{c2}"])
            P.op("vector", lambda e: e.tensor_tensor(qsA2[c2][:], QKt[:, 0:4:2, csl], Erow2[:], ALU.mult), reads=allq + ["Erow2"], writes=[f"qsA2_{c2}"])
            P.op("gpsimd", lambda e: e.tensor_tensor(qsB2[c2][0:64], QKt[0:64, 1:4:2, csl], Erow2[0:64], ALU.mult), reads=allq + ["Erow2"], writes=[f"qsB2_{c2}"])
        elif sub == 4:
            for h in range(2):
                O, Dk = OB[h], DB[h]
                for o4 in range(4):
                    oc = slice(o4 * 64, (o4 + 1) * 64)
                    dst = PS[0:64, O * 512 + o4 * 128:O * 512 + (o4 + 1) * 128]
                    P.op("tensor", lambda e, dst=dst, oc=oc, h=h: e.matmul(dst, lhsT=vat[:, cc, h, oc], rhs=scT2[c2][:, h, :], start=True, stop=False),
                         reads=[f"vaug{par}_{cc}", f"vaug{par}", f"scT2_{c2}"], writes=[bkey(O)])
                    P.op("tensor", lambda e, dst=dst, oc=oc, h=h: e.matmul(dst, lhsT=Cb[h][0][:, oc], rhs=qsA2[c2][:, h, :], start=False, stop=False),
                         reads=[f"Cb_{h}0", f"qsA2_{c2}"], writes=[bkey(O)])
                    P.op("tensor", lambda e, dst=dst, oc=oc, h=h: e.matmul(dst, lhsT=Cb[h][1][0:64, oc], rhs=qsB2[c2][0:64, h, :], start=False, stop=True),
                         reads=[f"Cb_{h}1", f"qsB2_{c2}"], writes=[bkey(O)])
                dA = PS[:, Dk * 512:Dk * 512 + 256]
                dB = PS[0:64, Dk * 512 + 256:Dk * 512 + 512]
                P.op("tensor", lambda e, dA=dA, h=h: e.matmul(dA, lhsT=kw2[c2][:, h, 0:128], rhs=vat[:, cc, h, :], start=True, stop=True), reads=[f"kw2_{c2}", f"vaug{par}_{cc}", f"vaug{par}"], writes=[bkey(Dk)])
                P.op("tensor", lambda e, dB=dB, h=h: e.matmul(dB, lhsT=kw2[c2][:, h, 128:192], rhs=vat[:, cc, h, :], start=True, stop=True), reads=[f"kw2_{c2}", f"vaug{par}_{cc}", f"vaug{par}"], writes=[bkey(Dk)])
        elif sub == 5:
            for h in range(2):
                Dk = DB[h]
                dA = PS[:, Dk * 512:Dk * 512 + 256]
                dB = PS[0:64, Dk * 512 + 256:Dk * 512 + 512]
                P.op("vector", lambda e, dA=dA, h=h: e.scalar_tensor_tensor(C32[h][0][:], C32[h][0][:], dec2[c2][:, h:h + 1], dA, ALU.mult, ALU.add), reads=[bkey(Dk), f"dec2_{c2}"], writes=[f"C32_{h}0"])
                P.op("vector", lambda e, dB=dB, h=h: e.scalar_tensor_tensor(C32[h][1][0:64, :], C32[h][1][0:64, :], dec2[c2][0:64, h:h + 1], dB, ALU.mult, ALU.add), reads=[bkey(Dk), f"dec2_{c2}"], writes=[f"C32_{h}1"])
                P.op("scalar", lambda e, h=h: e.copy(Cb[h][0][:], C32[h][0][:]), reads=[f"C32_{h}0"], writes=[f"Cb_{h}0"])
                P.op("scalar", lambda e, h=h: e.copy(Cb[h][1][0:64, :], C32[h][1][0:64, :]), reads=[f"C32_{h}1"], writes=[f"Cb_{h}1"])
            den2 = PS[0:64, OB[0] * 512:(OB[0] + 2) * 512].rearrange("p (h c) -> p h c", h=2)[:, :, 384:512]
            P.op("scalar", lambda e: e.activation(rden2[0:64], den2, AF.Abs), reads=[bkey(OB[0]), bkey(OB[1])], writes=["rden2"])
            P.op("vector", lambda e: e.tensor_scalar(rden2[0:64], rden2[0:64], 1.0, None, ALU.max), reads=["rden2"], writes=["rden2"])
            P.op("vector", lambda e: e.reciprocal(rden2[0:64], rden2[0:64]), reads=["rden2"], writes=["rden2"])
        elif sub == 6:
            for p3 in range(3):
                num2 = PS[0:64, OB[0] * 512:(OB[0] + 2) * 512].rearrange("p (h c) -> p h c", h=2)[:, :, p3 * 128:(p3 + 1) * 128]
                P.op("gpsimd", lambda e, p3=p3: e.tensor_tensor(ogr2[0:64, p3], ogt[0:64, p3:6:3, csl], rden2[0:64], ALU.mult),
                     reads=[f"og{par}_{p3}", f"og{par}_{p3 + 3}", "rden2"], writes=[f"ogr2_{p3}"])
                P.op("vector", lambda e, p3=p3, num2=num2: e.tensor_tensor(yco2[0:64, :, p3, csl], num2, ogr2[0:64, p3], ALU.mult),
                     reads=[bkey(OB[0]), bkey(OB[1]), f"ogr2_{p3}"], writes=[f"yco_{p3}"])

    for g_ in proj_groups(0):
        g_()
    chunks = [(tt, cc) for tt in range(NTA) for cc in range(4)]
    nxt = []
    for sub in range(4):
        sub_step(0, 0, sub)
    for ci_, (tt, cc) in enumerate(chunks):
        if cc == 0:
            nxt = proj_groups(tt + 1) if tt + 1 < NTA else []
        nc_ = chunks[ci_ + 1] if ci_ + 1 < len(chunks) else None
        if nc_ is not None and nc_[1] == 0:
            for g_ in nxt:
                g_()
            nxt = []
        order = [(0, True), (4, False), (1, True), (5, False), (2, True), (6, False), (3, True)]
        for sub, is_next in order:
            if is_next:
                if nc_ is not None:
                    sub_step(nc_[0], nc_[1], sub)
            else:
                sub_step(tt, cc, sub)
            if nxt:
                nxt.pop(0)()
        if cc != 3:
            continue
        for h in range(2):
            for p3 in range(3):
                q = 3 * h + p3
                P.op("sync", lambda e, h=h, p3=p3, q=q, tt=tt: e.dma_start(out=T["ysend"][tt // 4].ap()[128 + 64 * q:128 + 64 * q + 64, (tt % 4) * TT:(tt % 4 + 1) * TT], in_=yco2[0:64, h, p3, :]),
                     reads=[f"yco_{p3}"], writes=["out_yc"], dma_key="outA")
    P.wait_res("sync", ["out_yc"])
    P.pop()
    P.pop()


BF = ml_dtypes.bfloat16
OFF = {}
_sizes = (768, 768, 768, 768, 768, 1536, 768, 768, 4, 4, 3072)
_names = ("a_q", "a_k", "a_v", "b_u", "b_v", "c_qk", "c_v", "c_o", "c_i", "c_f", "g")
_o = 0
for n_, s_ in zip(_names, _sizes):
    OFF[n_] = _o
    _o += s_


def const_tables():
    ident = np.eye(128, dtype=np.float32)
    triU = np.triu(np.ones((128, 128), np.float32))
    sel8 = np.zeros((8, 8 * 128), np.float32)
    for e in range(8):
        sel8[e, e * 128:(e + 1) * 128] = 1.0
    return dict(ident=ident, triU=triU, sel8=sel8)


def prep_B_weights(inp, layer):
    w_in, b_in = inp["w_in"][layer], inp["b_in"][layer]
    cols = np.r_[OFF["b_u"]:OFF["b_u"] + 768, OFF["b_v"]:OFF["b_v"] + 768, OFF["g"]:OFF["g"] + 3072]
    d = {}
    d["wB"] = np.ascontiguousarray(w_in[:, cols])
    d["bU"] = np.ascontiguousarray(b_in[OFF["b_u"]:OFF["b_u"] + 768].reshape(6, 128).T)
    d["bV"] = np.ascontiguousarray(b_in[OFF["b_v"]:OFF["b_v"] + 768].reshape(1, 768))
    d["bG"] = np.ascontiguousarray(b_in[OFF["g"]:OFF["g"] + 3072].reshape(24, 128).T)
    d["sgT"] = np.ascontiguousarray(inp["sg_w"][layer].transpose(0, 2, 1))
    d["sgb"] = np.ascontiguousarray(inp["sg_b"][layer].reshape(1, 768))
    d["slg"] = np.ascontiguousarray(inp["sg_ln_g"][layer].reshape(1, 768))
    d["slb"] = np.ascontiguousarray(inp["sg_ln_b"][layer].reshape(1, 768))
    wbr = np.concatenate([inp["w_br_a"][layer], inp["w_br_b"][layer], inp["w_br_c"][layer]], axis=0).reshape(14, 128, 1024)
    d["wbr"] = wbr
    d["wo"] = np.ascontiguousarray(inp["w_out"][layer])
    d["lng"] = np.ascontiguousarray(inp["ln_g"][layer].reshape(16, 128).T)
    d["lnb"] = np.ascontiguousarray(inp["ln_b"][layer].reshape(16, 128).T)
    j = layer // 2
    if layer % 2 == 0:
        d["w1"], d["w3"], d["w2"] = inp["ffn_w1"][j:j + 1], inp["ffn_w3"][j:j + 1], inp["ffn_w2"][j:j + 1]
    else:
        d["w1"], d["w3"], d["w2"] = inp["moe_w1"][j], inp["moe_w3"][j], inp["moe_w2"][j]
        d["rw"] = np.ascontiguousarray(inp["router_w"][j])
        d["rb"] = np.ascontiguousarray(inp["router_b"][j].reshape(1, 8))
    d.update(const_tables())
    return d


def yc_pieces_from_tokenmajor(y_c):
    n = y_c.shape[0]
    out = np.zeros((8, 128, n), y_c.dtype)
    for h in range(4):
        out[2 * h] = y_c[:, h * 192:h * 192 + 128].T
        out[2 * h + 1, 0:64] = y_c[:, h * 192 + 128:h * 192 + 192].T
    return out


def const_tables_A():
    p = np.arange(128)
    d = p % 64
    idx = (d % 32).astype(np.float32)
    invf = (np.float32(10000.0) ** (-idx * np.float32(2.0 / 64))).astype(np.float32).reshape(128, 1)
    sgn = np.where(d < 32, -1.0, 1.0).astype(np.float32).reshape(128, 1)
    perm = np.where(d < 32, p + 32, p - 32)
    permM = np.zeros((128, 128), np.float32)
    permM[perm, p] = 1.0
    triU = np.triu(np.ones((128, 128), np.float32))
    triL = np.tril(np.ones((128, 128), np.float32))
    maskA = np.concatenate([triU, triL, triU, triL], axis=1)
    return dict(ident=np.eye(128, dtype=np.float32), triU=triU, permM=permM, invf=invf, sgn=sgn, maskA=maskA)


def _pad128(v):
    out = np.zeros(128, np.float32)
    out[:v.shape[0]] = v
    return out


def prep_A_weights(inp, layer, hh):
    w_in, b_in = inp["w_in"][layer], inp["b_in"][layer]
    cols = []
    for g in range(3):
        h0 = 4 * g + 2 * hh
        for nm in ("a_q", "a_k", "a_v"):
            cols += list(range(OFF[nm] + h0 * 64, OFF[nm] + h0 * 64 + 128))
    m0 = 2 * hh
    cols += list(range(OFF["c_qk"] + m0 * 192, OFF["c_qk"] + m0 * 192 + 384))
    cols += list(range(OFF["c_qk"] + 768 + m0 * 192, OFF["c_qk"] + 768 + m0 * 192 + 384))
    cols += list(range(OFF["c_v"] + m0 * 192, OFF["c_v"] + m0 * 192 + 384))
    cols += list(range(OFF["c_o"] + m0 * 192, OFF["c_o"] + m0 * 192 + 384))
    cols += [OFF["c_i"] + m0, OFF["c_i"] + m0 + 1, OFF["c_f"] + m0, OFF["c_f"] + m0 + 1]
    cols = np.array(cols)
    assert cols.shape[0] == 2692
    d = {}
    d["wA"] = np.ascontiguousarray(w_in[:, cols])
    bA = b_in[cols]
    d["bqk"] = np.ascontiguousarray(np.stack([bA[g * 384 + w * 128:g * 384 + (w + 1) * 128] for g in range(3) for w in range(2)], axis=1))
    d["bva"] = np.ascontiguousarray(np.concatenate([bA[g * 384 + 256:g * 384 + 384] for g in range(3)]).reshape(1, 384))
    POFF, PW = [0, 128, 192, 320], [128, 64, 128, 64]
    d["bml"] = np.ascontiguousarray(np.stack([_pad128(bA[1152 + w * 384 + POFF[pc]:1152 + w * 384 + POFF[pc] + PW[pc]]) for w in range(2) for pc in range(4)], axis=1))
    d["bvm"] = np.ascontiguousarray(bA[1152 + 768:1152 + 1152].reshape(1, 384))
    d["bo"] = np.ascontiguousarray(np.stack([_pad128(bA[1152 + 1152 + q * 64:1152 + 1152 + (q + 1) * 64]) for q in range(6)], axis=1))
    d["bif"] = np.ascontiguousarray(bA[2688:2692].reshape(1, 4))
    cwl, cbl = inp["conv_w"][layer], inp["conv_b"][layer]
    cw = np.zeros((128, 8, 4), np.float32)
    cb = np.zeros((128, 8), np.float32)
    for w in range(2):
        for pc in range(4):
            ch0 = w * 768 + m0 * 192 + POFF[pc]
            cw[:PW[pc], w * 4 + pc, :] = cwl[:, ch0:ch0 + PW[pc]].T
            cb[:PW[pc], w * 4 + pc] = cbl[ch0:ch0 + PW[pc]]
    d["cw"], d["cb"] = cw, cb
    d.update(const_tables_A())
    return d


PAIRS = [[0, 1], [2, 3], [4, 5], [6, 7]]
A_LAYER = dict(wA=([1024, 2692], F32), bqk=([128, 6], F32), bva=([1, 384], F32), bml=([128, 8], F32), bvm=([1, 384], F32),
               bif=([1, 4], F32), cw=([128, 8, 4], F32), cb=([128, 8], F32), bo=([128, 6], F32))
B_LAYER = dict(wB=([1024, 4608], F32), bU=([128, 6], F32), bV=([1, 768], F32), bG=([128, 24], F32),
               sgT=([6, 128, 128], F32), sgb=([1, 768], F32), slg=([1, 768], F32), slb=([1, 768], F32),
               wbr=([14, 128, 1024], F32), wo=([1024, 1024], F32), lng=([128, 16], F32), lnb=([128, 16], F32))
CONSTS = dict(ident=([128, 128], F32), triU=([128, 128], F32), permM=([128, 128], F32), invf=([128, 1], F32), sgn=([128, 1], F32),
              maskA=([128, 512], F32), sel8=([8, 1024], F32))


def _input_shapes():
    sh = dict(x0T=([1024, 4096], F32), x0h=([1024, 2048], F32), pos=([1, 4096], I32), selh=([128, 2], F32))
    sh.update(CONSTS)
    for l in range(2):
        for k, v in A_LAYER.items():
            sh[f"{k}_A{l}"] = v
        for k, v in B_LAYER.items():
            sh[f"{k}_B{l}"] = v
        E = 8 if l == 1 else 1
        sh[f"w1_B{l}"] = ([E, 1024, 2816], F32)
        sh[f"w3_B{l}"] = ([E, 1024, 2816], F32)
        sh[f"w2_B{l}"] = ([E, 2816, 1024], F32)
    sh["rw_B1"] = ([1024, 8], F32)
    sh["rb_B1"] = ([1, 8], F32)
    return sh


def _build_fused():
    nc = bass.Bass("TRN2", target_bir_lowering=False)
    shapes = _input_shapes()
    I = {k: nc.dram_tensor(k, s, dt, kind="ExternalInput") for k, (s, dt) in shapes.items()}
    out = nc.dram_tensor("out", [1024, 2048], F32, kind="ExternalOutput")
    ysend = [nc.dram_tensor(f"ysend{i}", [512, 2048], BF16) for i in range(2)]
    ygath = [nc.dram_tensor(f"ygath{i}", [1024, 2048], BF16) for i in range(2)]
    x1s = nc.dram_tensor("x1_scr", [1024, 2048], F32)
    xbs = [nc.dram_tensor(f"xbs{i}", [512, 2048], BF16) for i in range(2)]
    xg = [nc.dram_tensor(f"xg{i}", [1024, 2048], BF16) for i in range(2)]

    def allgather(P, src, dst, reads, writes, key):
        P.op("gpsimd", lambda e: e.collective_compute("AllGather", ALU.bypass, replica_groups=PAIRS, ins=[src.ap().opt()], outs=[dst.ap().opt()]),
             reads=reads, writes=writes, dma_key=key, sem_inc=1)

    with ExitStack() as st:
        P = Prog(nc, st)

        def load_wuv(l, buf):
            wB_ = I[f"wB_B{l}"].ap()
            for c in range(8):
                P.op("gpsimd", lambda e, c=c: e.dma_start(out=buf[:, c, :], in_=wB_[c * 128:(c + 1) * 128, 0:1536]),
                     writes=[f"wUV{c}"], dma_key=f"wUVld{c}")

        for l in range(2):
            TA = {k: I[k] for k in CONSTS}
            TA.update({k: I[f"{k}_A{l}"] for k in A_LAYER})
            TA["pos"] = I["pos"]
            TA["xsrc"] = ("ext", I["x0T"]) if l == 0 else ("gath", xg[0], xg[1])
            TA["ysend"] = ysend
            build_A(nc, P, TA, tag=f"A{l}")
            TB = {k: I[k] for k in CONSTS}
            TB.update({k: I[f"{k}_B{l}"] for k in B_LAYER})
            for k in ("w1", "w3", "w2"):
                TB[k] = I[f"{k}_B{l}"]
            if l == 1:
                TB["rw"], TB["rb"] = I["rw_B1"], I["rb_B1"]
            TB["selh"] = I["selh"]
            TB["ygath"] = ygath
            TB["x_in"] = I["x0h"] if l == 0 else x1s
            TB["xo"] = x1s if l == 0 else out
            TB["xob"] = xbs if l == 0 else None
            P.push()
            P.prefix = f"BL{l}_"
            wuv_l = P.sb([128, 8, 1536], BF16, name="wuv")
            for i in range(2):
                allgather(P, ysend[i], ygath[i], ["out_ya", "out_yc"], [f"ygath{i}"], f"ccy{i}")
            load_wuv(l, wuv_l)
            TB["wuv_buf"] = wuv_l
            for half in range(2):
                build_B(nc, P, TB, l == 1, half, tag=f"B{l}")
            P.pop()
            if l == 0:
                for i in range(2):
                    allgather(P, xbs[i], xg[i], ["out_xb"], ["xg"], f"ccx{i}")
        P.wait_res("sync", ["out_x"])
        P.emit()
    return nc, list(shapes.keys())


def kernel(**inputs):
    inp = {k: np.asarray(v) for k, v in inputs.items()}
    cores = list(range(8))
    nc, names = _build_fused()
    consts = const_tables_A()
    consts["sel8"] = const_tables()["sel8"]
    wA = [[prep_A_weights(inp, l, hh) for hh in range(2)] for l in range(2)]
    wBs = [prep_B_weights(inp, l) for l in range(2)]
    maps = []
    for c in cores:
        b, r = c // 2, c % 2
        xT = np.ascontiguousarray(inp["x"][b].T)
        m = dict(x0T=xT, x0h=np.ascontiguousarray(xT[:, r * 2048:(r + 1) * 2048]),
                 pos=np.ascontiguousarray(inp["positions"][b:b + 1]).astype(np.int32))
        sel = np.zeros((128, 2), np.float32)
        sel[:, r] = 1.0
        m["selh"] = sel
        for k in CONSTS:
            m[k] = consts[k]
        for l in range(2):
            for k in A_LAYER:
                m[f"{k}_A{l}"] = wA[l][r][k]
            for k in list(B_LAYER) + ["w1", "w3", "w2"]:
                m[f"{k}_B{l}"] = wBs[l][k]
        m["rw_B1"], m["rb_B1"] = wBs[1]["rw"], wBs[1]["rb"]
        maps.append({k: np.ascontiguousarray(m[k]) for k in names})
    res = run_bass_kernel_spmd(nc, maps, core_ids=cores).results
    out = np.empty((4, 4096, 1024), np.float32)
    for c in cores:
        b, r = c // 2, c % 2
        out[b, r * 2048:(r + 1) * 2048, :] = res[c]["out"].T
    return out
```
